# Optimizing a Trainium2 kernel written in Bass

```python
import math
import jax
import jax.numpy as jnp
from jax import lax
import numpy as np

D_MODEL = 4096
BATCH = 4
SEQ = 2048
DEPTH = 2

F32 = jnp.float32
EPS = 1e-6
NEG_INF = -1e30

N_MIXERS = 4
MIX_WIDTH = D_MODEL
GROUP_WIDTH = MIX_WIDTH // N_MIXERS

S5_CH_PER_GROUP = 16
S5_GROUPS = GROUP_WIDTH // S5_CH_PER_GROUP
S5_STATE = 64

DIFF_HEADS = 8
DIFF_HEAD_DIM = GROUP_WIDTH // (2 * DIFF_HEADS)
Q_BLOCK = 128

MOBA_HEADS = 8
MOBA_HEAD_DIM = GROUP_WIDTH // MOBA_HEADS
MOBA_BLOCK = 256
MOBA_TOPK = 3
MOBA_Q_CHUNK = 32

SSD_HEAD_DIM = 64
SSD_HEADS = GROUP_WIDTH // SSD_HEAD_DIM
SSD_GROUPS = 4
SSD_STATE = 128
SSD_CONV = 4
SSD_CHUNK = 128
SSD_BC = SSD_GROUPS * SSD_STATE
SSD_XBC = GROUP_WIDTH + 2 * SSD_BC

REL_BUCKETS = 32
REL_MAX_DIST = 128
ATTN_HEADS = DIFF_HEADS + MOBA_HEADS

D_FF = 4 * D_MODEL

OFF_S5 = 0
OFF_DIFF = OFF_S5 + GROUP_WIDTH
OFF_MOBA = OFF_DIFF + 3 * GROUP_WIDTH
OFF_SSD = OFF_MOBA + 3 * GROUP_WIDTH
IN_WIDTH = OFF_SSD + GROUP_WIDTH + SSD_XBC + SSD_HEADS

kernel_name = 'hybrid_parallel_heads_s5_diffattn_moba_ssd'


def rms_norm(x, w):
    xf = x.astype(F32)
    y = xf * lax.rsqrt(jnp.mean(xf * xf, axis=-1, keepdims=True) + EPS)
    return (y * w.astype(F32)).astype(x.dtype)


def rel_bucket(dist):
    n = jnp.maximum(dist, 0)
    max_exact = REL_BUCKETS // 2
    log_ratio = jnp.log(jnp.maximum(n, 1).astype(F32) / max_exact) / math.log(REL_MAX_DIST / max_exact)
    large = max_exact + (log_ratio * (REL_BUCKETS - max_exact)).astype(jnp.int32)
    large = jnp.minimum(large, REL_BUCKETS - 1)
    return jnp.where(n < max_exact, n, large)


def _s5_combine(e1, e2):
    a1r, a1i, b1r, b1i = e1
    a2r, a2i, b2r, b2i = e2
    ar = a2r * a1r - a2i * a1i
    ai = a2r * a1i + a2i * a1r
    br = a2r * b1r - a2i * b1i + b2r
    bi = a2r * b1i + a2i * b1r + b2i
    return ar, ai, br, bi


def s5_mixer(u, lam_re, lam_im, log_dt, b_re, b_im, c_re, c_im, d_skip, w_glu):
    bsz, seq, _ = u.shape
    G, Hc, P = S5_GROUPS, S5_CH_PER_GROUP, S5_STATE
    uf = u.astype(F32).reshape(bsz, seq, G, Hc)
    dt = jnp.exp(log_dt.astype(F32))[:, None]
    lr, li = lam_re.astype(F32), lam_im.astype(F32)
    mag = jnp.exp(lr * dt)
    ab_re = mag * jnp.cos(li * dt)
    ab_im = mag * jnp.sin(li * dt)
    den = lr * lr + li * li
    f_re = ((ab_re - 1.0) * lr + ab_im * li) / den
    f_im = (ab_im * lr - (ab_re - 1.0) * li) / den
    br, bi = b_re.astype(F32), b_im.astype(F32)
    bb_re = f_re[..., None] * br - f_im[..., None] * bi
    bb_im = f_re[..., None] * bi + f_im[..., None] * br
    bu_re = jnp.einsum('bsgh,gph->bsgp', uf, bb_re)
    bu_im = jnp.einsum('bsgh,gph->bsgp', uf, bb_im)
    a_re = jnp.broadcast_to(ab_re, (1, seq, G, P))
    a_im = jnp.broadcast_to(ab_im, (1, seq, G, P))
    _, _, s_re, s_im = lax.associative_scan(_s5_combine, (a_re, a_im, bu_re, bu_im), axis=1)
    y = (jnp.einsum('bsgp,ghp->bsgh', s_re, c_re.astype(F32))
         - jnp.einsum('bsgp,ghp->bsgh', s_im, c_im.astype(F32))
         + d_skip.astype(F32) * uf)
    y = jax.nn.gelu(y.reshape(bsz, seq, GROUP_WIDTH)).astype(u.dtype)
    return y * jax.nn.sigmoid(y @ w_glu)


def diff_attention(q, k, v, lam_q1, lam_k1, lam_q2, lam_k2, subln_w, rel_table, layer_idx):
    bsz, seq, _ = q.shape
    H, dh = DIFF_HEADS, DIFF_HEAD_DIM
    q = q.reshape(bsz, seq, H, 2, dh).transpose(0, 2, 3, 1, 4)
    k = k.reshape(bsz, seq, H, 2, dh).transpose(0, 2, 3, 1, 4)
    v = v.reshape(bsz, seq, H, 2 * dh).transpose(0, 2, 1, 3)
    lam_init = 0.8 - 0.6 * math.exp(-0.3 * layer_idx)
    lam = (jnp.exp(jnp.sum(lam_q1.astype(F32) * lam_k1.astype(F32)))
           - jnp.exp(jnp.sum(lam_q2.astype(F32) * lam_k2.astype(F32))) + lam_init)
    scale = dh ** -0.5
    k_pos = jnp.arange(seq)
    tbl = rel_table[:, :H].astype(F32)
    nqb = seq // Q_BLOCK
    q_blocks = jnp.moveaxis(q.reshape(bsz, H, 2, nqb, Q_BLOCK, dh), 3, 0)

    def one_block(args):
        qi, qb = args
        q_pos = qi * Q_BLOCK + jnp.arange(Q_BLOCK)
        logits = jnp.einsum('bhmqd,bhmkd->bhmqk', qb, k).astype(F32) * scale
        bias = jnp.transpose(tbl[rel_bucket(q_pos[:, None] - k_pos[None, :])], (2, 0, 1))
        causal = k_pos[None, :] <= q_pos[:, None]
        logits = jnp.where(causal, logits + bias[None, :, None], NEG_INF)
        p = jax.nn.softmax(logits, axis=-1)
        w = p[:, :, 0] - lam * p[:, :, 1]
        return jnp.einsum('bhqk,bhkd->bhqd', w.astype(v.dtype), v)

    out = lax.map(one_block, (jnp.arange(nqb), q_blocks))
    out = jnp.moveaxis(out, 0, 2).reshape(bsz, H, seq, 2 * dh)
    out = rms_norm(out, subln_w) * (1.0 - lam_init)
    return out.transpose(0, 2, 1, 3).reshape(bsz, seq, H * 2 * dh)


def moba_attention(q, k, v, rel_table):
    bsz, seq, _ = q.shape
    H, dh, BLK, QC = MOBA_HEADS, MOBA_HEAD_DIM, MOBA_BLOCK, MOBA_Q_CHUNK
    q = q.reshape(bsz, seq, H, dh).transpose(0, 2, 1, 3)
    k = k.reshape(bsz, seq, H, dh).transpose(0, 2, 1, 3)
    v = v.reshape(bsz, seq, H, dh).transpose(0, 2, 1, 3)
    nb = -(-seq // BLK)
    pad = nb * BLK - seq
    k = jnp.pad(k, ((0, 0), (0, 0), (0, pad), (0, 0)))
    v = jnp.pad(v, ((0, 0), (0, 0), (0, pad), (0, 0)))
    k_blocks = k.reshape(bsz, H, nb, BLK, dh)
    v_blocks = v.reshape(bsz, H, nb, BLK, dh)
    k_mean = jnp.mean(k_blocks.astype(F32), axis=3)
    topk = max(1, min(MOBA_TOPK, nb - 1))
    tbl = rel_table[:, DIFF_HEADS:].astype(F32).T
    scale = dh ** -0.5
    b_idx = jnp.arange(bsz)[:, None, None, None]
    h_idx = jnp.arange(H)[None, :, None, None]
    offs = jnp.arange(BLK)
    nqc = seq // QC
    q_chunks = jnp.moveaxis(q.reshape(bsz, H, nqc, QC, dh), 2, 0)

    def one_chunk(args):
        ci, qc = args
        q_pos = ci * QC + jnp.arange(QC)
        own = (ci * QC) // BLK
        gate = jnp.einsum('bhqd,bhnd->bhqn', qc.astype(F32), k_mean)
        gate = jnp.where(jnp.arange(nb) < own, gate, NEG_INF)
        _, sel = lax.top_k(gate, topk)
        valid = sel < own
        k_sel = k_blocks[b_idx, h_idx, sel]
        v_sel = v_blocks[b_idx, h_idx, sel]
        k_pos_sel = sel[..., None] * BLK + offs
        logit_sel = jnp.einsum('bhqd,bhqtkd->bhqtk', qc, k_sel).astype(F32) * scale
        logit_sel = logit_sel + tbl[h_idx[..., None], rel_bucket(q_pos[:, None, None] - k_pos_sel)]
        logit_sel = jnp.where(valid[..., None], logit_sel, NEG_INF).reshape(bsz, H, QC, topk * BLK)
        k_own = lax.dynamic_slice_in_dim(k, own * BLK, BLK, axis=2)
        v_own = lax.dynamic_slice_in_dim(v, own * BLK, BLK, axis=2)
        k_pos_own = own * BLK + offs
        logit_own = jnp.einsum('bhqd,bhkd->bhqk', qc, k_own).astype(F32) * scale
        logit_own = logit_own + tbl[:, rel_bucket(q_pos[:, None] - k_pos_own[None, :])][None]
        logit_own = jnp.where(k_pos_own[None, :] <= q_pos[:, None], logit_own, NEG_INF)
        p = jax.nn.softmax(jnp.concatenate([logit_sel, logit_own], axis=-1), axis=-1).astype(v.dtype)
        p_sel, p_own = p[..., :topk * BLK], p[..., topk * BLK:]
        return (jnp.einsum('bhqt,bhqtd->bhqd', p_sel, v_sel.reshape(bsz, H, QC, topk * BLK, dh))
                + jnp.einsum('bhqk,bhkd->bhqd', p_own, v_own))

    out = lax.map(one_chunk, (jnp.arange(nqc), q_chunks))
    out = jnp.moveaxis(out, 0, 2).reshape(bsz, H, seq, dh)
    return out.transpose(0, 2, 1, 3).reshape(bsz, seq, H * dh)


def ssd_chunked(x, a, b, c):
    bsz, seq, H, P = x.shape
    G, N, Q = SSD_GROUPS, SSD_STATE, SSD_CHUNK
    R = H // G
    nc = seq // Q
    x = x.reshape(bsz, nc, Q, G, R, P)
    b = b.reshape(bsz, nc, Q, G, N)
    c = c.reshape(bsz, nc, Q, G, N)
    a = a.reshape(bsz, nc, Q, G, R).transpose(0, 3, 4, 1, 2)
    a_cum = jnp.cumsum(a, axis=-1)
    causal = jnp.tril(jnp.ones((Q, Q), dtype=bool))
    seg = jnp.where(causal, a_cum[..., :, None] - a_cum[..., None, :], NEG_INF)
    decay_in = jnp.exp(seg)
    y_diag = jnp.einsum('bclgn,bcsgn,bgrcls,bcsgrp->bclgrp', c, b, decay_in, x)
    decay_to_end = jnp.exp(a_cum[..., -1:] - a_cum)
    chunk_states = jnp.einsum('bclgn,bgrcl,bclgrp->bcgrpn', b, decay_to_end, x)
    chunk_decay = jnp.exp(a_cum[..., -1])

    def step(h, inp):
        s, d = inp
        return h * d[..., None, None] + s, h

    h0 = jnp.zeros((bsz, G, R, P, N), F32)
    _, prev = lax.scan(step, h0, (jnp.moveaxis(chunk_states, 1, 0), jnp.moveaxis(chunk_decay, 3, 0)))
    prev = jnp.moveaxis(prev, 0, 1)
    y_off = jnp.einsum('bclgn,bcgrpn,bgrcl->bclgrp', c, prev, jnp.exp(a_cum))
    return (y_diag + y_off).reshape(bsz, seq, H, P)


def ssd_mixer(z, xbc, dt_raw, conv_w, conv_b, dt_bias, a_log, d_skip, norm_w):
    bsz, seq, _ = z.shape
    xbc = lax.conv_general_dilated(xbc, conv_w, window_strides=(1,), padding=[(SSD_CONV - 1, 0)],
                                   dimension_numbers=('NWC', 'WIO', 'NWC'),
                                   feature_group_count=SSD_XBC) + conv_b
    xbc = jax.nn.silu(xbc)
    xs = xbc[..., :GROUP_WIDTH].reshape(bsz, seq, SSD_HEADS, SSD_HEAD_DIM).astype(F32)
    bs = xbc[..., GROUP_WIDTH:GROUP_WIDTH + SSD_BC].reshape(bsz, seq, SSD_GROUPS, SSD_STATE).astype(F32)
    cs = xbc[..., GROUP_WIDTH + SSD_BC:].reshape(bsz, seq, SSD_GROUPS, SSD_STATE).astype(F32)
    dt = jax.nn.softplus(dt_raw.astype(F32) + dt_bias.astype(F32))
    a = -jnp.exp(a_log.astype(F32))
    y = ssd_chunked(xs * dt[..., None], dt * a, bs, cs)
    y = y + d_skip.astype(F32)[:, None] * xs
    y = y.reshape(bsz, seq, GROUP_WIDTH) * jax.nn.silu(z.astype(F32))
    y = rms_norm(y.reshape(bsz, seq, SSD_GROUPS, GROUP_WIDTH // SSD_GROUPS),
                 norm_w.reshape(SSD_GROUPS, GROUP_WIDTH // SSD_GROUPS))
    return y.reshape(bsz, seq, GROUP_WIDTH).astype(z.dtype)


def setup_inputs(seed: int = 0) -> dict:
    key = jax.random.key(seed)
    keys = iter(list(jax.random.split(key, 40)))
    L = DEPTH

    def nrm(shape, scale):
        return jax.random.normal(next(keys), shape, F32) * scale

    def gain(shape):
        return 1.0 + nrm(shape, 0.02)

    x = nrm((BATCH, SEQ, D_MODEL), 1.0)
    rel_bias_table = nrm((REL_BUCKETS, ATTN_HEADS), 0.2)
    attn_norm_w = gain((L, D_MODEL))
    w_in = nrm((L, D_MODEL, IN_WIDTH), D_MODEL ** -0.5)
    s5_lam_re = -0.5 + nrm((L, S5_GROUPS, S5_STATE), 0.01)
    s5_lam_im = math.pi * jnp.arange(S5_STATE, dtype=F32) + nrm((L, S5_GROUPS, S5_STATE), 0.01)
    s5_log_dt = jax.random.uniform(next(keys), (L, S5_GROUPS), F32, math.log(1e-3), math.log(1e-1))
    s5_b_re = nrm((L, S5_GROUPS, S5_STATE, S5_CH_PER_GROUP), S5_CH_PER_GROUP ** -0.5)
    s5_b_im = nrm((L, S5_GROUPS, S5_STATE, S5_CH_PER_GROUP), S5_CH_PER_GROUP ** -0.5)
    s5_c_re = nrm((L, S5_GROUPS, S5_CH_PER_GROUP, S5_STATE), S5_STATE ** -0.5)
    s5_c_im = nrm((L, S5_GROUPS, S5_CH_PER_GROUP, S5_STATE), S5_STATE ** -0.5)
    s5_d = nrm((L, S5_GROUPS, S5_CH_PER_GROUP), 0.5)
    s5_w_glu = nrm((L, GROUP_WIDTH, GROUP_WIDTH), GROUP_WIDTH ** -0.5)
    s5_out_norm_w = gain((L, GROUP_WIDTH))
    diff_lam_q1 = nrm((L, DIFF_HEAD_DIM), 0.1)
    diff_lam_k1 = nrm((L, DIFF_HEAD_DIM), 0.1)
    diff_lam_q2 = nrm((L, DIFF_HEAD_DIM), 0.1)
    diff_lam_k2 = nrm((L, DIFF_HEAD_DIM), 0.1)
    diff_subln_w = gain((L, 2 * DIFF_HEAD_DIM))
    moba_out_norm_w = gain((L, GROUP_WIDTH))
    ssd_conv_w = nrm((L, SSD_CONV, 1, SSD_XBC), SSD_CONV ** -0.5)
    ssd_conv_b = nrm((L, SSD_XBC), 0.01)
    dt0 = jnp.exp(jax.random.uniform(next(keys), (L, SSD_HEADS), F32, math.log(1e-3), math.log(1e-1)))
    ssd_dt_bias = dt0 + jnp.log(-jnp.expm1(-dt0))
    ssd_a_log = jnp.log(jax.random.uniform(next(keys), (L, SSD_HEADS), F32, 1.0, 16.0))
    ssd_d = gain((L, SSD_HEADS))
    ssd_norm_w = gain((L, GROUP_WIDTH))
    w_out = nrm((L, MIX_WIDTH, D_MODEL), MIX_WIDTH ** -0.5)
    mlp_norm_w = gain((L, D_MODEL))
    w_up = nrm((L, D_MODEL, D_FF), D_MODEL ** -0.5)
    w_down = nrm((L, D_FF, D_MODEL), D_FF ** -0.5)
    final_norm_w = gain((D_MODEL,))
    return {'x': x, 'rel_bias_table': rel_bias_table, 'attn_norm_w': attn_norm_w, 'w_in': w_in,
            's5_lam_re': s5_lam_re, 's5_lam_im': s5_lam_im, 's5_log_dt': s5_log_dt,
            's5_b_re': s5_b_re, 's5_b_im': s5_b_im, 's5_c_re': s5_c_re, 's5_c_im': s5_c_im,
            's5_d': s5_d, 's5_w_glu': s5_w_glu, 's5_out_norm_w': s5_out_norm_w,
            'diff_lam_q1': diff_lam_q1, 'diff_lam_k1': diff_lam_k1, 'diff_lam_q2': diff_lam_q2,
            'diff_lam_k2': diff_lam_k2, 'diff_subln_w': diff_subln_w, 'moba_out_norm_w': moba_out_norm_w,
            'ssd_conv_w': ssd_conv_w, 'ssd_conv_b': ssd_conv_b, 'ssd_dt_bias': ssd_dt_bias,
            'ssd_a_log': ssd_a_log, 'ssd_d': ssd_d, 'ssd_norm_w': ssd_norm_w, 'w_out': w_out,
            'mlp_norm_w': mlp_norm_w, 'w_up': w_up, 'w_down': w_down, 'final_norm_w': final_norm_w}


def reference(x, rel_bias_table, attn_norm_w, w_in, s5_lam_re, s5_lam_im, s5_log_dt, s5_b_re, s5_b_im,
              s5_c_re, s5_c_im, s5_d, s5_w_glu, s5_out_norm_w, diff_lam_q1, diff_lam_k1, diff_lam_q2,
              diff_lam_k2, diff_subln_w, moba_out_norm_w, ssd_conv_w, ssd_conv_b, ssd_dt_bias, ssd_a_log,
              ssd_d, ssd_norm_w, w_out, mlp_norm_w, w_up, w_down, final_norm_w):
    for l in range(DEPTH):
        h = rms_norm(x, attn_norm_w[l])
        proj = h @ w_in[l]
        u_s5 = proj[..., OFF_S5:OFF_DIFF]
        dq, dk, dv = jnp.split(proj[..., OFF_DIFF:OFF_MOBA], 3, axis=-1)
        mq, mk, mv = jnp.split(proj[..., OFF_MOBA:OFF_SSD], 3, axis=-1)
        ssd_in = proj[..., OFF_SSD:]
        z = ssd_in[..., :GROUP_WIDTH]
        xbc = ssd_in[..., GROUP_WIDTH:GROUP_WIDTH + SSD_XBC]
        dt_raw = ssd_in[..., GROUP_WIDTH + SSD_XBC:]

        y_s5 = rms_norm(s5_mixer(u_s5, s5_lam_re[l], s5_lam_im[l], s5_log_dt[l], s5_b_re[l], s5_b_im[l],
                                 s5_c_re[l], s5_c_im[l], s5_d[l], s5_w_glu[l]), s5_out_norm_w[l])
        y_diff = diff_attention(dq, dk, dv, diff_lam_q1[l], diff_lam_k1[l], diff_lam_q2[l], diff_lam_k2[l],
                                diff_subln_w[l], rel_bias_table, l)
        y_moba = rms_norm(moba_attention(mq, mk, mv, rel_bias_table), moba_out_norm_w[l])
        y_ssd = ssd_mixer(z, xbc, dt_raw, ssd_conv_w[l], ssd_conv_b[l], ssd_dt_bias[l], ssd_a_log[l],
                          ssd_d[l], ssd_norm_w[l])

        mixed = jnp.concatenate([y_s5, y_diff, y_moba, y_ssd], axis=-1)
        x = x + mixed @ w_out[l]
        h = rms_norm(x, mlp_norm_w[l])
        x = x + jnp.square(jax.nn.relu(h @ w_up[l])) @ w_down[l]
    return rms_norm(x, final_norm_w)
```

```python
import numpy as np
import concourse.bass as bass
import concourse.mybir as mybir
from concourse.bass_utils import run_bass_kernel_spmd

AF = mybir.ActivationFunctionType
ALU = mybir.AluOpType
AX = mybir.AxisListType
F32 = mybir.dt.float32
F32R = mybir.dt.float32r
BF16 = mybir.dt.bfloat16
I32 = mybir.dt.int32

SAME_ENG_SYNC = True


_UNIQ = [0]


def _sbt(nc, name, shape, dtype):
    _UNIQ[0] += 1
    return nc.sbuf_tensor("%s_u%d" % (name, _UNIQ[0]), shape, dtype)


class Tok:
    __slots__ = ("name", "w", "r")

    def __init__(self, name):
        self.name = name
        self.w = None
        self.r = []


class Sched:
    NDS = 48

    def __init__(self, nc, stack):
        self.nc = nc
        self.engs = {"pe": nc.tensor, "act": nc.scalar, "dve": nc.vector,
                     "pool": nc.gpsimd, "sp": nc.sync}
        self.sem = {}
        for e in ("pe", "act", "dve", "pool"):
            self.sem[e] = stack.enter_context(nc.semaphore("sem_" + e))
        self.cnt = {e: 0 for e in self.sem}
        self.dsem = [stack.enter_context(nc.semaphore("dsem%d" % i)) for i in range(self.NDS)]
        self.dval = [0] * self.NDS
        self.dnext = 0
        self.seen = {e: {} for e in self.engs}
        self.ninstr = 0

    def buf(self, name):
        return Tok(name)

    def bufs(self, name, n):
        return [Tok("%s%d" % (name, i)) for i in range(n)]

    def _wait(self, eng, tok):
        if tok is None:
            return
        kind, key, val = tok
        if kind == "eng" and key == eng and not (SAME_ENG_SYNC and eng in ("act", "dve", "pool")):
            return
        skey = (kind, key)
        if self.seen[eng].get(skey, 0) >= val:
            return
        sem = self.sem[key] if kind == "eng" else self.dsem[key]
        self.engs[eng].wait_ge(sem, val)
        self.seen[eng][skey] = val

    def _deps(self, eng, reads, writes):
        for t in reads:
            self._wait(eng, t.w)
        for t in writes:
            self._wait(eng, t.w)
            for r in t.r:
                self._wait(eng, r)

    def _mark(self, tok, reads, writes):
        for t in reads:
            t.r.append(tok)
            if len(t.r) > 12:
                d = {}
                for r in t.r:
                    k = (r[0], r[1])
                    if k not in d or d[k][2] < r[2]:
                        d[k] = r
                t.r = list(d.values())
        for t in writes:
            t.w = tok
            t.r = []

    def op(self, eng, fn, reads=(), writes=()):
        self._deps(eng, reads, writes)
        ins = fn()
        self.cnt[eng] += 1
        ins.then_inc(self.sem[eng], 1)
        tok = ("eng", eng, self.cnt[eng])
        self._mark(tok, reads, writes)
        self.ninstr += 1
        return tok

    def group(self, eng, fns, reads=(), writes=()):
        self._deps(eng, reads, writes)
        ins = None
        for f in fns:
            ins = f()
            self.ninstr += 1
        self.cnt[eng] += 1
        ins.then_inc(self.sem[eng], 1)
        tok = ("eng", eng, self.cnt[eng])
        self._mark(tok, reads, writes)
        return tok

    def dma(self, q, out, in_, reads=(), writes=(), **kw):
        slot = self.dnext
        self.dnext = (self.dnext + 1) % self.NDS
        if self.dval[slot] > 0:
            self._wait(q, ("dma", slot, self.dval[slot]))
        self._deps(q, reads, writes)
        self.dval[slot] += 16
        self.engs[q].dma_start(out=out, in_=in_, **kw).then_inc(self.dsem[slot], 16)
        tok = ("dma", slot, self.dval[slot])
        self._mark(tok, reads, writes)
        self.ninstr += 1
        return tok

    def wait_tok(self, eng, tok):
        self._wait(eng, tok)

    def barrier(self):
        for e in self.engs:
            for e2 in self.sem:
                if self.cnt[e2] > 0:
                    self._wait(e, ("eng", e2, self.cnt[e2]))
            for s in range(self.NDS):
                if self.dval[s] > 0:
                    self._wait(e, ("dma", s, self.dval[s]))


from contextlib import ExitStack

D = 4096
KC = 32
INW = 10256
EPS = 1e-6


class Ctx:
    def __init__(self, nc, st):
        self.nc = nc
        self.st = st
        self.S = Sched(nc, st)
        self.ps = st.enter_context(nc.psum_tensor("ps", [128, 4096], F32))
        self.PS = self.S.bufs("psbank", 8)
        self.ident = st.enter_context(_sbt(nc, "ident", [128, 128], BF16))
        self.identf = st.enter_context(_sbt(nc, "identf", [128, 128], F32))
        self.IDT = self.S.buf("ident")

    def bank(self, b, n=512):
        return self.ps[:, b * 512:b * 512 + n]

    def bank_bf(self, b):
        return self.ps[:, b * 512:(b + 1) * 512].bitcast(BF16)


def load_consts(C, ident_d):
    nc, S = C.nc, C.S
    S.dma("sp", C.identf[:], ident_d[:, :], writes=[C.IDT])
    S.op("dve", lambda: nc.vector.tensor_copy(out=C.ident[:], in_=C.identf[:]), reads=[C.IDT], writes=[C.IDT])


def build_hT(C, x_rows, nw_d, T, hT, HT, norm=True):
    nc, S = C.nc, C.S
    NT = T // 128
    with ExitStack() as st:
        xs = [st.enter_context(_sbt(nc, "xs%d" % i, [128, D], F32)) for i in range(2)]
        XS = S.bufs("xs", 2)
        junk = st.enter_context(_sbt(nc, "junk", [128, D], BF16))
        JK = S.buf("junk")
        xnb = [st.enter_context(_sbt(nc, "xnb%d" % i, [128, D], BF16)) for i in range(2)]
        XNB = S.bufs("xnb", 2)
        stat = st.enter_context(_sbt(nc, "stat", [128, 4 * NT], F32))
        STT = S.bufs("stat", NT)
        nwT = st.enter_context(_sbt(nc, "nwT", [128, KC], F32))
        NW = S.buf("nwT")
        if norm:
            with nc.allow_non_contiguous_dma(reason="small norm weight transpose"):
                S.dma("sp", nwT[:], nw_d.rearrange("(j p) -> p j", p=128), writes=[NW])
        for tt in range(NT):
            i = tt % 2
            S.dma("sp", xs[i][:], x_rows[tt * 128:(tt + 1) * 128, :], writes=[XS[i]])
            ss = stat[:, 4 * tt:4 * tt + 1]
            sd = stat[:, 4 * tt + 1:4 * tt + 2]
            rs = stat[:, 4 * tt + 2:4 * tt + 3]
            if norm:
                S.op("act", lambda: nc.scalar.activation(out=junk[:], in_=xs[i][:], func=AF.Square, accum_out=ss),
                     reads=[XS[i]], writes=[JK, STT[tt]])
                S.op("act", lambda: nc.scalar.activation(out=sd, in_=ss, func=AF.Sqrt, scale=1.0 / D, bias=EPS),
                     reads=[STT[tt]], writes=[STT[tt]])
                S.op("dve", lambda: nc.vector.reciprocal(out=rs, in_=sd), reads=[STT[tt]], writes=[STT[tt]])
                S.op("act", lambda: nc.scalar.activation(out=xnb[i][:], in_=xs[i][:], func=AF.Copy, scale=rs),
                     reads=[XS[i], STT[tt]], writes=[XNB[i]])
            else:
                S.op("act", lambda: nc.scalar.copy(out=xnb[i][:], in_=xs[i][:]), reads=[XS[i]], writes=[XNB[i]])
            for g in range(4):
                b = (tt % 2) * 4 + g
                pb = C.bank_bf(b)
                fns = []
                for jj in range(8):
                    j = g * 8 + jj
                    fns.append(lambda j=j, jj=jj, pb=pb: nc.tensor.transpose(
                        pb[:, jj * 128:(jj + 1) * 128], xnb[i][:, j * 128:(j + 1) * 128], C.ident[:]))
                S.group("pe", fns, reads=[XNB[i], C.IDT], writes=[C.PS[b]])
                if norm:
                    S.op("dve", lambda g=g, pb=pb: nc.vector.tensor_tensor(
                        out=hT[:, g * 8:(g + 1) * 8, tt * 128:(tt + 1) * 128],
                        in0=pb.rearrange("p (j t) -> p j t", j=8),
                        in1=nwT[:, g * 8:(g + 1) * 8].unsqueeze(2).to_broadcast([128, 8, 128]),
                        op=ALU.mult), reads=[C.PS[b], NW], writes=[HT])
                else:
                    S.op("dve", lambda g=g, pb=pb: nc.vector.tensor_copy(
                        out=hT[:, g * 8:(g + 1) * 8, tt * 128:(tt + 1) * 128],
                        in_=pb.rearrange("p (j t) -> p j t", j=8)), reads=[C.PS[b]], writes=[HT])
        S.barrier()


def dense(C, AT, ATK, kc_n, T, w_d, blocks, evac, name="d", pre=None, a_d=None, bg=None):
    nc, S = C.nc, C.S
    KG = 4
    ngr = kc_n // KG
    NH = T // 512
    NT = T // 128
    with ExitStack() as st:
        NB = 5 if a_d is None else 4
        ws = [st.enter_context(_sbt(nc, "%s_ws%d" % (name, i), [128, KG, 512], F32)) for i in range(NB)]
        wb = [st.enter_context(_sbt(nc, "%s_wb%d" % (name, i), [128, KG, 512], BF16)) for i in range(NB)]
        WS = S.bufs(name + "ws", NB)
        WB = S.bufs(name + "wb", NB)
        if a_d is not None:
            ab = [st.enter_context(_sbt(nc, "%s_ab%d" % (name, i), [128, KG, T], BF16)) for i in range(NB)]
            AB = S.bufs(name + "ab", NB)
            av = a_d.rearrange("(c p) t -> p c t", p=128)
        it = 0
        wv = w_d.rearrange("(c p) n -> p c n", p=128)
        for (c0, width, mode) in blocks:
            if mode == "fm":
                mts = [(m0, min(128, width - m0)) for m0 in range(0, width, 128)]
                assert len(mts) * NH <= 8
            else:
                assert NT <= 8
            if pre is not None:
                pre(c0)
            for g in range(ngr):
                i = it % NB
                it += 1
                if bg is not None:
                    bg()
                S.dma("sp", ws[i][:, :, 0:width], wv[:, g * KG:(g + 1) * KG, c0:c0 + width], writes=[WS[i]])
                if a_d is not None:
                    S.dma("sp", ab[i][:], av[:, g * KG:(g + 1) * KG, :], writes=[AB[i]])
                if it % 2 == 0:
                    S.op("act", lambda i=i: nc.scalar.copy(out=wb[i][:, :, 0:width], in_=ws[i][:, :, 0:width]),
                         reads=[WS[i]], writes=[WB[i]])
                else:
                    S.op("dve", lambda i=i: nc.vector.tensor_copy(out=wb[i][:, :, 0:width], in_=ws[i][:, :, 0:width]),
                         reads=[WS[i]], writes=[WB[i]])
                per_bank = {}
                for j in range(KG):
                    kc = g * KG + j
                    first = (kc == 0)
                    last = (kc == kc_n - 1)
                    if mode == "fm":
                        for mi, (m0, mw) in enumerate(mts):
                            for h in range(NH):
                                bnk = mi * NH + h
                                per_bank.setdefault(bnk, []).append(
                                    lambda i=i, j=j, m0=m0, mw=mw, h=h, bnk=bnk, kc=kc, first=first, last=last:
                                    nc.tensor.matmul(C.bank(bnk)[0:mw, :], wb[i][:, j, m0:m0 + mw],
                                                     AT[:, kc, h * 512:(h + 1) * 512], start=first, stop=last))
                    else:
                        for tt in range(NT):
                            if a_d is None:
                                per_bank.setdefault(tt, []).append(
                                    lambda i=i, j=j, tt=tt, kc=kc, first=first, last=last:
                                    nc.tensor.matmul(C.bank(tt)[:, 0:width], AT[:, kc, tt * 128:(tt + 1) * 128],
                                                     wb[i][:, j, 0:width], start=first, stop=last))
                            else:
                                per_bank.setdefault(tt, []).append(
                                    lambda i=i, j=j, tt=tt, kc=kc, first=first, last=last:
                                    nc.tensor.matmul(C.bank(tt)[:, 0:width], ab[i][:, j, tt * 128:(tt + 1) * 128],
                                                     wb[i][:, j, 0:width], start=first, stop=last))
                rd = [WB[i]] + ([ATK] if a_d is None else [AB[i]])
                if g == 0:
                    for bnk in sorted(per_bank):
                        S.group("pe", per_bank[bnk], reads=rd, writes=[C.PS[bnk]])
                else:
                    fns = []
                    nb_ = sorted(per_bank)
                    for j in range(KG):
                        for bnk in nb_:
                            fns.append(per_bank[bnk][j])
                    S.group("pe", fns, reads=rd, writes=[C.PS[bnk] for bnk in nb_])
            if mode == "fm":
                for mi, (m0, mw) in enumerate(mts):
                    for h in range(NH):
                        bnk = mi * NH + h
                        evac(mode, c0, (m0, mw, h), C.bank(bnk)[0:mw, :], C.PS[bnk])
            else:
                for tt in range(NT):
                    evac(mode, c0, tt, C.bank(tt)[:, 0:width], C.PS[tt])
        S.barrier()


class Stager:
    def __init__(self, C, st, name, n=4, dtype=F32, width=512):
        self.C = C
        self.t = [st.enter_context(_sbt(C.nc, "%s%d" % (name, i), [128, width], dtype)) for i in range(n)]
        self.T = C.S.bufs(name, n)
        self.i = 0
        self.n = n

    def next(self):
        i = self.i % self.n
        self.i += 1
        return self.t[i], self.T[i], ("dve" if self.i % 2 == 0 else "act")


def copy_op(C, eng, out, in_, reads, writes):
    nc, S = C.nc, C.S
    if eng == "act":
        return S.op("act", lambda: nc.scalar.copy(out=out, in_=in_), reads=reads, writes=writes)
    elif eng == "dve":
        return S.op("dve", lambda: nc.vector.tensor_copy(out=out, in_=in_), reads=reads, writes=writes)
    else:
        return S.op("pool", lambda: nc.gpsimd.tensor_copy(out=out, in_=in_), reads=reads, writes=writes)


def win_blocks():
    bl = []
    fm_ranges = [(0, 3072), (4096, 6144), (8192, 10240)]
    tm_ranges = [(3072, 4096), (6144, 8192)]
    for a, b in fm_ranges:
        for c in range(a, b, 512):
            bl.append((c, 512, "fm"))
    for a, b in tm_ranges:
        for c in range(a, b, 512):
            bl.append((c, 512, "tm"))
    bl.append((10240, 16, "fm"))
    return bl


def phase_A(C, x_rows, nw_d, w_d, T, tok0, projF, projT, blocks=None, bg=None):
    nc, S = C.nc, C.S
    with ExitStack() as st:
        hT = st.enter_context(_sbt(nc, "hT", [128, KC, T], BF16))
        HT = S.buf("hT")
        build_hT(C, x_rows, nw_d, T, hT, HT)
        stg = Stager(C, st, "stgA", 8)

        def evac(mode, c0, idx, pap, ptok):
            t, tk, eng = stg.next()
            if mode == "fm":
                m0, mw, h = idx
                copy_op(C, eng, t[0:mw, :], pap, [ptok], [tk])
                S.dma("pool", projF[c0 + m0:c0 + m0 + mw, tok0 + h * 512:tok0 + (h + 1) * 512], t[0:mw, :], reads=[tk])
            else:
                tt = idx
                w = pap.shape[1]
                copy_op(C, eng, t[:, 0:w], pap, [ptok], [tk])
                S.dma("pool", projT[tok0 + tt * 128:tok0 + (tt + 1) * 128, c0:c0 + w], t[:, 0:w], reads=[tk])

        dense(C, hT, HT, KC, T, w_d, blocks or win_blocks(), evac, name="A", bg=bg)


def load_AT(C, src_d, kc_n, T, t0, AT, ATK, name="ld"):
    nc, S = C.nc, C.S
    sv = src_d.rearrange("(c p) t -> p c t", p=128)
    with ExitStack() as st:
        tmp = [st.enter_context(_sbt(nc, "%s_t%d" % (name, i), [128, 4, T], F32)) for i in range(2)]
        TM = S.bufs(name + "t", 2)
        for g in range(kc_n // 4):
            i = g % 2
            S.dma("sp", tmp[i][:], sv[:, g * 4:(g + 1) * 4, t0:t0 + T], writes=[TM[i]])
            copy_op(C, ["act", "pool", "dve"][g % 3], AT[:, g * 4:(g + 1) * 4, :], tmp[i][:], [TM[i]], [ATK])
        S.barrier()


def phase_C(C, mixT_d, x_d, wout_d, nw2_d, wup_d, wdown_d, T, tok0, xmid_d, act_d, xout_d,
            final_nw_d=None, out_d=None, mixed_tm=None):
    nc, S = C.nc, C.S
    FF = 16384
    with ExitStack() as st:
        mT = st.enter_context(_sbt(nc, "mT", [128, KC, T], BF16))
        MT = S.buf("mT")
        if mixed_tm is not None:
            build_hT(C, mixed_tm[tok0:tok0 + T, :], None, T, mT, MT, norm=False)
        else:
            load_AT(C, mixT_d, KC, T, tok0, mT, MT, name="ldm")
        stg = Stager(C, st, "stgC", 8)
        xin = Stager(C, st, "xinC", 8)
        pend = {}

        def pre1(c0):
            for tt in range(T // 128):
                xi, xk, _ = xin.next()
                rows = slice(tok0 + tt * 128, tok0 + (tt + 1) * 128)
                S.dma("pool", xi[:], x_d[rows, c0:c0 + 512], writes=[xk])
                pend[tt] = (xi, xk)

        def evac1(mode, c0, tt, pap, ptok):
            t, tk, _ = stg.next()
            xi, xk = pend[tt]
            rows = slice(tok0 + tt * 128, tok0 + (tt + 1) * 128)
            if tt % 2 == 0:
                S.op("dve", lambda: nc.vector.tensor_tensor(out=t[:], in0=pap, in1=xi[:], op=ALU.add),
                     reads=[ptok, xk], writes=[tk])
            else:
                S.op("act", lambda: nc.scalar.copy(out=t[:], in_=pap), reads=[ptok], writes=[tk])
                S.op("pool", lambda: nc.gpsimd.tensor_tensor(out=t[:], in0=t[:], in1=xi[:], op=ALU.add), reads=[tk, xk], writes=[tk])
            S.dma("pool", xmid_d[rows, c0:c0 + 512], t[:], reads=[tk])

        dense(C, mT, MT, KC, T, wout_d, [(c, 512, "tm") for c in range(0, D, 512)], evac1, name="O", pre=pre1)
    with ExitStack() as st:
        h2T = st.enter_context(_sbt(nc, "h2T", [128, KC, T], BF16))
        H2 = S.buf("h2T")
        build_hT(C, xmid_d[tok0:tok0 + T, :], nw2_d, T, h2T, H2)
        stg = Stager(C, st, "stgU", 8)
        stb = Stager(C, st, "stbU", 8, dtype=BF16)

        def evac2(mode, c0, idx, pap, ptok):
            m0, mw, h = idx
            t, tk, _ = stg.next()
            tb, tbk, _ = stb.next()
            if stg.i % 2 == 0:
                S.op("act", lambda: nc.scalar.activation(out=t[:], in_=pap, func=AF.Relu), reads=[ptok], writes=[tk])
            else:
                S.op("dve", lambda: nc.vector.tensor_scalar(out=t[:], in0=pap, scalar1=0.0, scalar2=None, op0=ALU.max), reads=[ptok], writes=[tk])
            S.op("pool", lambda: nc.gpsimd.tensor_tensor(out=tb[:], in0=t[:], in1=t[:], op=ALU.mult), reads=[tk], writes=[tbk])
            S.dma("pool", act_d[c0 + m0:c0 + m0 + mw, h * 512:(h + 1) * 512], tb[:], reads=[tbk])

        dense(C, h2T, H2, KC, T, wup_d, [(c, 512, "fm") for c in range(0, FF, 512)], evac2, name="U")
    with ExitStack() as st:
        stg = Stager(C, st, "stgD", 8)
        xin = Stager(C, st, "xinD", 8)
        pend3 = {}

        def pre3(c0):
            for tt in range(T // 128):
                xi, xk, _ = xin.next()
                rows = slice(tok0 + tt * 128, tok0 + (tt + 1) * 128)
                S.dma("pool", xi[:], xmid_d[rows, c0:c0 + 512], writes=[xk])
                pend3[tt] = (xi, xk)

        def evac3(mode, c0, tt, pap, ptok):
            t, tk, _ = stg.next()
            xi, xk = pend3[tt]
            rows = slice(tok0 + tt * 128, tok0 + (tt + 1) * 128)
            if tt % 2 == 0:
                S.op("dve", lambda: nc.vector.tensor_tensor(out=t[:], in0=pap, in1=xi[:], op=ALU.add),
                     reads=[ptok, xk], writes=[tk])
            else:
                S.op("act", lambda: nc.scalar.copy(out=t[:], in_=pap), reads=[ptok], writes=[tk])
                S.op("pool", lambda: nc.gpsimd.tensor_tensor(out=t[:], in0=t[:], in1=xi[:], op=ALU.add), reads=[tk, xk], writes=[tk])
            S.dma("pool", xout_d[rows, c0:c0 + 512], t[:], reads=[tk])

        dense(C, None, None, FF // 128, T, wdown_d, [(c, 512, "tm") for c in range(0, D, 512)], evac3, name="Dn", pre=pre3, a_d=act_d)
    if final_nw_d is not None:
        final_norm(C, xout_d, final_nw_d, T, tok0, out_d)


def final_norm(C, xin_d, nw_d, T, tok0, out_d):
    nc, S = C.nc, C.S
    NT = T // 128
    with ExitStack() as st:
        xs = [st.enter_context(_sbt(nc, "fxs%d" % i, [128, D], F32)) for i in range(2)]
        XS = S.bufs("fxs", 2)
        ys = [st.enter_context(_sbt(nc, "fys%d" % i, [128, D], F32)) for i in range(2)]
        YS = S.bufs("fys", 2)
        nwb = st.enter_context(_sbt(nc, "fnw", [128, D], F32))
        NW = S.buf("fnw")
        stat = st.enter_context(_sbt(nc, "fstat", [128, 4 * NT], F32))
        STT = S.bufs("fstat", NT)
        S.dma("sp", nwb[:], nw_d.partition_broadcast(128), writes=[NW])
        for tt in range(NT):
            i = tt % 2
            rows = slice(tok0 + tt * 128, tok0 + (tt + 1) * 128)
            S.dma("sp", xs[i][:], xin_d[rows, :], writes=[XS[i]])
            ss = stat[:, 4 * tt:4 * tt + 1]
            sd = stat[:, 4 * tt + 1:4 * tt + 2]
            rs = stat[:, 4 * tt + 2:4 * tt + 3]
            S.op("act", lambda: nc.scalar.activation(out=ys[i][:], in_=xs[i][:], func=AF.Square, accum_out=ss),
                 reads=[XS[i]], writes=[YS[i], STT[tt]])
            S.op("act", lambda: nc.scalar.activation(out=sd, in_=ss, func=AF.Sqrt, scale=1.0 / D, bias=EPS),
                 reads=[STT[tt]], writes=[STT[tt]])
            S.op("dve", lambda: nc.vector.reciprocal(out=rs, in_=sd), reads=[STT[tt]], writes=[STT[tt]])
            S.op("dve", lambda: nc.vector.scalar_tensor_tensor(out=ys[i][:], in0=xs[i][:], scalar=rs, in1=nwb[:],
                                                               op0=ALU.mult, op1=ALU.mult),
                 reads=[XS[i], STT[tt], NW], writes=[YS[i]])
            S.dma("sp", out_d[rows, :], ys[i][:], reads=[YS[i]])
        S.barrier()


def host_consts():
    import math
    def bucket(n):
        n = max(n, 0)
        if n < 16:
            return n
        lr = np.log(np.float32(max(n, 1)) / np.float32(16)) / np.float32(math.log(128 / 16))
        return min(16 + int(np.float32(lr) * np.float32(16)), 31)
    bk = np.array([bucket(n) for n in range(0, 256)])
    ohm = np.zeros((2, 32, 128, 128), np.float32)
    kl = np.arange(128)[:, None]
    ql = np.arange(128)[None, :]
    for t in range(2):
        dist = t * 128 + ql - kl
        valid = dist >= 0
        b = bk[np.clip(dist, 0, 255)]
        for bb in range(32):
            ohm[t, bb] = ((b == bb) & valid).astype(np.float32)
        ohm[t, 31] -= valid.astype(np.float32)
    negmask = np.where(ql < kl, -30000.0, 0.0).astype(np.float32)
    sel = np.zeros((8, 8, 128), np.float32)
    for n in range(8):
        sel[n, n, :] = 1.0
    q = np.arange(128)
    m2c = np.stack([(q // 64 == 0), (q // 64 == 1)], 1).astype(np.float32)
    m3c = np.stack([((q // 16) % 2 == 0), ((q // 16) % 2 == 1)], 1).astype(np.float32)
    m4c = np.stack([(q // 32 == k) for k in range(4)], 1).astype(np.float32)
    return {"iota": np.arange(2048, dtype=np.float32), "m2c": m2c, "m3c": m3c, "m4c": m4c, "ohm": ohm.reshape(2, 32, 16384), "negmask": negmask, "ident_d": np.eye(128, dtype=np.float32),
            "sel": sel.reshape(8, 1024)}


def build_bias(C, tbl_d, ohm_d, btd):
    nc, S = C.nc, C.S
    with ExitStack() as st:
        tb = st.enter_context(_sbt(nc, "bb_tbl", [32, 16], F32))
        TB = S.buf("bb_tbl")
        oh = [st.enter_context(_sbt(nc, "bb_oh%d" % i, [32, 16384], F32)) for i in range(2)]
        OH = S.bufs("bb_oh", 2)
        stg = Stager(C, st, "bb_stg", 4)
        S.dma("sp", tb[:], tbl_d[:, :], writes=[TB])
        for t in range(2):
            S.dma("sp", oh[t][:], ohm_d[t, :, :], writes=[OH[t]])
        k = 0
        for t in range(2):
            for c in range(32):
                b = k % 8
                k += 1
                S.op("pe", lambda: nc.tensor.matmul(C.bank(b)[0:16, :], tb[:], oh[t][:, c * 512:(c + 1) * 512],
                                                    start=True, stop=True), reads=[TB, OH[t]], writes=[C.PS[b]])
                sg, sk, eng = stg.next()
                copy_op(C, eng, sg[0:16, :], C.bank(b)[0:16, :], [C.PS[b]], [sk])
                S.dma("sp", btd[t, :, c * 512:(c + 1) * 512], sg[0:16, :], reads=[sk])
        S.barrier()


def load_bias_tiles(C, btd, negmask_sb, NM, head, inv_scale, T0b, T1b, TB_tok, tmp, TMP):
    nc, S = C.nc, C.S
    for t, dst in ((0, T0b), (1, T1b)):
        S.dma("sp", tmp[:], btd[t, head, :].rearrange("(k q) -> k q", q=128), writes=[TMP])
        if t == 0:
            S.op("dve", lambda: nc.vector.tensor_tensor(out=tmp[:], in0=tmp[:], in1=negmask_sb[:], op=ALU.add),
                 reads=[TMP, NM], writes=[TMP])
        S.op("act", lambda: nc.scalar.mul(out=dst[:], in_=tmp[:], mul=inv_scale), reads=[TMP], writes=[TB_tok])


def attention(C, ptl, name, qT, kT, QK, vb, VB, dv, T0b, T1b, TBK, transform, on_O, extra=None, nqb=4):
    nc, S = C.nc, C.S
    pt, PT = ptl
    assert len(pt) >= 4
    SBANKS = (0, 1, 6)
    vw = vb.shape[2]
    iters = [(qb, kt) for qb in range(nqb) for kt in range(4 * qb + 4)]

    def emit_S(n):
        qb, kt = iters[n]
        qs0 = max(0, kt - 4 * qb)
        c0 = qs0 * 128
        sb = SBANKS[n % 3]
        bank = C.bank(sb)
        ops = [(bank[:, c0:512], kT[:, kt * 128:(kt + 1) * 128], qT[:, qb * 512 + c0:qb * 512 + 512])]
        rd = [QK]
        for qs in range(qs0, 4):
            d = 4 * qb + qs - kt
            if d == 0 and T0b is not None:
                ops.append((bank[:, qs * 128:(qs + 1) * 128], C.ident[:], T0b[:]))
                rd += [TBK, C.IDT]
            if d == 1 and T1b is not None:
                ops.append((bank[:, qs * 128:(qs + 1) * 128], C.ident[:], T1b[:]))
                rd += [TBK, C.IDT]
        if extra is not None:
            eo, er = extra(kt, qb, qs0, bank)
            ops += eo
            rd += er
        fns = []
        for oi, (o, l, r) in enumerate(ops):
            fns.append(lambda o=o, l=l, r=r, oi=oi: nc.tensor.matmul(o, l, r, start=(oi == 0), stop=(oi == len(ops) - 1)))
        S.group("pe", fns, reads=rd, writes=[C.PS[sb]])

    emit_S(0)
    if len(iters) > 1:
        emit_S(1)
    for n, (qb, kt) in enumerate(iters):
        if n + 2 < len(iters):
            emit_S(n + 2)
        qs0 = max(0, kt - 4 * qb)
        c0 = qs0 * 128
        sb = SBANKS[n % 3]
        i = n % 4
        bank = C.bank(sb)
        transform(kt, qb, c0, bank[:, c0:512], pt[i][:, c0:512], [C.PS[sb]], [PT[i]])
        fns = []
        wr = []
        for qs in range(qs0, 4):
            qt = 4 * qb + qs
            fns.append(lambda qs=qs, qt=qt: nc.tensor.matmul(C.bank(2 + qs)[:, 0:vw], pt[i][:, qs * 128:(qs + 1) * 128],
                                                            vb[:, kt, :], start=(kt == 0), stop=(kt == qt)))
            wr.append(C.PS[2 + qs])
        S.group("pe", fns, reads=[PT[i], VB], writes=wr)
        if kt >= 4 * qb:
            on_O(kt, C.bank(2 + (kt - 4 * qb))[:, 0:vw], C.PS[2 + (kt - 4 * qb)])


def exp_transform(C, scale):
    nc, S = C.nc, C.S

    def tr(kt, qb, c0, pap, oap, reads, writes):
        S.op("act", lambda: nc.scalar.activation(out=oap, in_=pap, func=AF.Exp, scale=scale), reads=reads, writes=writes)
    return tr


def rstd_ops(C, ss, sd, rs, n, TK):
    nc, S = C.nc, C.S
    S.op("dve", lambda: nc.vector.tensor_scalar(out=sd, in0=ss, scalar1=1.0 / n, scalar2=EPS, op0=ALU.mult, op1=ALU.add), reads=[TK], writes=[TK])
    S.op("act", lambda: nc.scalar.activation(out=sd, in_=sd, func=AF.Ln), reads=[TK], writes=[TK])
    S.op("act", lambda: nc.scalar.activation(out=rs, in_=sd, func=AF.Exp, scale=-0.5), reads=[TK], writes=[TK])


def diff_attn(C, layer, projF, projT, btd, negmask_d, lam_d, subw_d, mixed_d):
    import math
    nc, S = C.nc, C.S
    lam_init = 0.8 - 0.6 * math.exp(-0.3 * layer)
    scale = 64 ** -0.5
    with ExitStack() as st:
        sb = lambda n, s, d=F32: st.enter_context(_sbt(nc, "df_" + n, s, d))
        negm = sb("negm", [128, 128]); NM = S.buf("negm")
        S.dma("sp", negm[:], negmask_d[:, :], writes=[NM])
        lv = sb("lv", [128, 4, 64]); LV = S.buf("lv")
        for i in range(4):
            S.dma("sp", lv[:, i, :], lam_d[i].partition_broadcast(128), writes=[LV])
        ls = sb("ls", [128, 8]); LS = S.buf("ls")
        junk = sb("junk", [128, 64])
        for m in range(2):
            S.op("dve", lambda: nc.vector.tensor_tensor(out=junk[:], in0=lv[:, 2 * m, :], in1=lv[:, 2 * m + 1, :], op=ALU.mult),
                 reads=[LV], writes=[LS])
            S.op("dve", lambda: nc.vector.tensor_reduce(out=ls[:, m:m + 1], in_=junk[:], axis=AX.X, op=ALU.add), reads=[LS], writes=[LS])
            S.op("act", lambda: nc.scalar.activation(out=ls[:, 2 + m:3 + m], in_=ls[:, m:m + 1], func=AF.Exp), reads=[LS], writes=[LS])
        S.op("dve", lambda: nc.vector.tensor_tensor(out=ls[:, 4:5], in0=ls[:, 3:4], in1=ls[:, 2:3], op=ALU.subtract), reads=[LS], writes=[LS])
        S.op("dve", lambda: nc.vector.tensor_scalar(out=ls[:, 5:6], in0=ls[:, 4:5], scalar1=-lam_init, scalar2=None, op0=ALU.add),
             reads=[LS], writes=[LS])
        nlam = ls[:, 5:6]
        subw = sb("subw", [128, 128]); SW = S.buf("subw")
        S.dma("sp", subw[:], subw_d.partition_broadcast(128), writes=[SW])
        S.op("act", lambda: nc.scalar.mul(out=subw[:], in_=subw[:], mul=(1.0 - lam_init)), reads=[SW], writes=[SW])
        qf = sb("qf", [128, 2048]); QF = S.buf("qf")
        kf = sb("kf", [128, 2048]); KF = S.buf("kf")
        vf = sb("vf", [128, 16, 128]); VF = S.buf("vf")
        qT = sb("qT", [128, 2048], BF16)
        kTm = [sb("kT%d" % m, [128, 2048], BF16) for m in range(2)]
        QK = S.buf("qk")
        vb = sb("vb", [128, 16, 129], BF16); VB = S.buf("vb")
        T0b = sb("T0b", [128, 128], BF16); T1b = sb("T1b", [128, 128], BF16); TBK = S.buf("tb")
        tmp = sb("tmp", [128, 128]); TMP = S.buf("tmp")
        o1 = sb("o1", [128, 16, 128]); O1 = S.buf("o1")
        yb = [sb("y%d" % i, [128, 128]) for i in range(2)]; YB = S.bufs("dfy", 2)
        stt = sb("stt", [128, 8 * 32]); STT = S.buf("stt")
        S.op("pool", lambda: nc.gpsimd.memset(vb[:, :, 128:129], 1.0), writes=[VB])
        ptl = ([sb("pt%d" % i, [128, 512], BF16) for i in range(4)], S.bufs("df_pt", 4))
        for m in range(2):
            S.op("pool", lambda: nc.gpsimd.memset(kTm[m][:], 0.0), writes=[QK])
        for h in range(8):
            S.dma("sp", qf[:], projF[1024 + h * 128:1024 + (h + 1) * 128, :], writes=[QF])
            S.dma("sp", kf[:], projF[2048 + h * 128:2048 + (h + 1) * 128, :], writes=[KF])
            S.dma("sp", vf[:], projT[:, 3072 + h * 128:3072 + (h + 1) * 128].rearrange("(t p) d -> p t d", p=128), writes=[VF])
            S.op("act", lambda: nc.scalar.copy(out=qT[:], in_=qf[:]), reads=[QF], writes=[QK])
            S.op("dve", lambda: nc.vector.tensor_copy(out=kTm[0][0:64, :], in_=kf[0:64, :]), reads=[KF], writes=[QK])
            S.op("dve", lambda: nc.vector.tensor_copy(out=kTm[1][64:128, :], in_=kf[64:128, :]), reads=[KF], writes=[QK])
            S.op("pool", lambda: nc.gpsimd.tensor_copy(out=vb[:, :, 0:128], in_=vf[:]), reads=[VF], writes=[VB])
            load_bias_tiles(C, btd, negm, NM, h, 1.0 / scale, T0b, T1b, TBK, tmp, TMP)
            for m in range(2):
                def on_O(qt, oap, otok, m=m, h=h):
                    c = (qt % 32) * 8
                    rc = stt[:, c:c + 1]
                    S.op("dve", lambda: nc.vector.reciprocal(out=rc, in_=oap[:, 128:129]), reads=[otok], writes=[STT])
                    if m == 0:
                        S.op("act", lambda: nc.scalar.activation(out=o1[:, qt, :], in_=oap[:, 0:128], func=AF.Copy, scale=rc),
                             reads=[otok, STT], writes=[O1])
                    else:
                        y = yb[qt % 2]; Y = YB[qt % 2]
                        r2 = stt[:, c + 1:c + 2]
                        S.op("dve", lambda: nc.vector.tensor_tensor(out=r2, in0=rc, in1=nlam, op=ALU.mult), reads=[STT, LS], writes=[STT])
                        S.op("dve", lambda: nc.vector.scalar_tensor_tensor(out=y[:], in0=oap[:, 0:128], scalar=r2, in1=o1[:, qt, :],
                                                                           op0=ALU.mult, op1=ALU.add), reads=[otok, STT, O1], writes=[Y])
                        ss = stt[:, c + 2:c + 3]; sd = stt[:, c + 3:c + 4]; rs = stt[:, c + 4:c + 5]
                        S.op("dve", lambda: nc.vector.tensor_tensor(out=tmp[:], in0=y[:], in1=y[:], op=ALU.mult), reads=[Y], writes=[TMP])
                        S.op("dve", lambda: nc.vector.tensor_reduce(out=ss, in_=tmp[:], axis=AX.X, op=ALU.add), reads=[TMP], writes=[STT])
                        rstd_ops(C, ss, sd, rs, 128, STT)
                        S.op("dve", lambda: nc.vector.scalar_tensor_tensor(out=y[:], in0=y[:], scalar=rs, in1=subw[:],
                                                                           op0=ALU.mult, op1=ALU.mult), reads=[Y, STT, SW], writes=[Y])
                        S.dma("sp", mixed_d[qt * 128:(qt + 1) * 128, 1024 + h * 128:1024 + (h + 1) * 128], y[:], reads=[Y])
                attention(C, ptl, "df%d_%d" % (h, m), qT, kTm[m], QK, vb, VB, 128, T0b, T1b, TBK,
                          exp_transform(C, scale), on_O)
        S.barrier()


def moba_attn(C, projF, projT, btd, negmask_d, sel_d, monw_d, mixed_d):
    nc, S = C.nc, C.S
    scale = 128 ** -0.5
    with ExitStack() as st:
        sb = lambda n, s, d=F32: st.enter_context(_sbt(nc, "mb_" + n, s, d))
        negm = sb("negm", [128, 128]); NM = S.buf("mb_negm")
        S.dma("sp", negm[:], negmask_d[:, :], writes=[NM])
        self = sb("self", [8, 1024]); selb = sb("selb", [128, 8, 128], BF16); SEL = S.buf("mb_sel")
        S.dma("sp", self[:], sel_d[:, :], writes=[SEL])
        S.op("pool", lambda: nc.gpsimd.memset(selb[:], 0.0), writes=[SEL])
        S.op("dve", lambda: nc.vector.tensor_copy(out=selb[0:8, :, :], in_=self[:].rearrange("p (n k) -> p n k", k=128)),
             reads=[SEL], writes=[SEL])
        monw = sb("monw", [128, 1024]); MW = S.buf("mb_monw")
        S.dma("sp", monw[:], monw_d.partition_broadcast(128), writes=[MW])
        qf = sb("qf", [128, 2048]); QF = S.buf("mb_qf")
        kf = sb("kf", [128, 2048]); KF = S.buf("mb_kf")
        vf = sb("vf", [128, 16, 128]); VF = S.buf("mb_vf")
        qT = sb("qT", [128, 2048], BF16); kT = sb("kT", [128, 2048], BF16); QK = S.buf("mb_qk")
        vb = sb("vb", [128, 16, 129], BF16); VB = S.buf("mb_vb")
        T0b = sb("T0b", [128, 128], BF16); T1b = sb("T1b", [128, 128], BF16); TBK = S.buf("mb_tb")
        tmp = sb("tmp", [128, 128]); TMP = S.buf("mb_tmp")
        km = sb("km", [128, 8]); KM = S.buf("mb_km")
        g8 = sb("g8", [128, 8]); mx = sb("mx", [128, 8]); mq = sb("mq", [128, 8]); G8 = S.buf("mb_g8")
        MTb = sb("MTb", [128, 2048], BF16); MT = S.buf("mb_MT")
        mo = sb("mo", [128, 16, 1024]); MO = S.buf("mb_mo")
        stt = sb("stt", [128, 64]); STT = S.buf("mb_stt")
        ptl = ([sb("pt%d" % i, [128, 512], BF16) for i in range(4)], S.bufs("mb_pt", 4))
        S.op("pool", lambda: nc.gpsimd.memset(vb[:, :, 128:129], 1.0), writes=[VB])
        S.op("pool", lambda: nc.gpsimd.memset(MTb[:], 0.0), writes=[MT])
        for h in range(8):
            S.dma("sp", qf[:], projF[4096 + h * 128:4096 + (h + 1) * 128, :], writes=[QF])
            S.dma("sp", kf[:], projF[5120 + h * 128:5120 + (h + 1) * 128, :], writes=[KF])
            S.dma("sp", vf[:], projT[:, 6144 + h * 128:6144 + (h + 1) * 128].rearrange("(t p) d -> p t d", p=128), writes=[VF])
            S.op("act", lambda: nc.scalar.copy(out=qT[:], in_=qf[:]), reads=[QF], writes=[QK])
            S.op("dve", lambda: nc.vector.tensor_copy(out=kT[:], in_=kf[:]), reads=[KF], writes=[QK])
            S.op("pool", lambda: nc.gpsimd.tensor_copy(out=vb[:, :, 0:128], in_=vf[:]), reads=[VF], writes=[VB])
            load_bias_tiles(C, btd, negm, NM, 8 + h, 1.0 / scale, T0b, T1b, TBK, tmp, TMP)
            S.op("dve", lambda: nc.vector.tensor_reduce(out=km[:], in_=kf[:].rearrange("p (n k) -> p n k", k=256), axis=AX.X, op=ALU.add),
                 reads=[KF], writes=[KM])
            for qt in range(8, 16):
                own = qt // 2
                S.op("pe", lambda: nc.tensor.matmul(C.bank(6)[:, 0:8], qf[:, qt * 128:(qt + 1) * 128], km[:], start=True, stop=True),
                     reads=[QF, KM], writes=[C.PS[6]])
                S.op("dve", lambda: nc.vector.tensor_copy(out=g8[:], in_=C.bank(6)[:, 0:8]), reads=[C.PS[6]], writes=[G8])
                S.op("dve", lambda: nc.vector.memset(g8[:, own:8], -1e30), reads=[], writes=[G8])
                S.op("dve", lambda: nc.vector.max(out=mx[:], in_=g8[:]), reads=[G8], writes=[G8])
                S.op("dve", lambda: nc.vector.tensor_scalar(out=mq[:], in0=g8[:], scalar1=mx[:, 2:3], scalar2=-30000.0,
                                                            op0=ALU.is_lt, op1=ALU.mult), reads=[G8], writes=[G8])
                S.op("pe", lambda: nc.tensor.transpose(C.bank(7)[0:8, 0:128], mq[:], C.identf[:]), reads=[G8, C.IDT], writes=[C.PS[7]])
                S.op("act", lambda: nc.scalar.copy(out=MTb[0:8, qt * 128:(qt + 1) * 128], in_=C.bank(7)[0:8, 0:128]),
                     reads=[C.PS[7]], writes=[MT])

            def extra(kt, qb, qs0, bank):
                n = kt // 2
                ops = []
                for qs in range(qs0, 4):
                    qt = 4 * qb + qs
                    if qt >= 8 and n < qt // 2:
                        ops.append((bank[:, qs * 128:(qs + 1) * 128], selb[:, n, :], MTb[:, qt * 128:(qt + 1) * 128]))
                return ops, [SEL, MT]

            def on_O(qt, oap, otok, h=h):
                rc = stt[:, qt:qt + 1]
                S.op("dve", lambda: nc.vector.reciprocal(out=rc, in_=oap[:, 128:129]), reads=[otok], writes=[STT])
                S.op("act", lambda: nc.scalar.activation(out=mo[:, qt, h * 128:(h + 1) * 128], in_=oap[:, 0:128], func=AF.Copy, scale=rc),
                     reads=[otok, STT], writes=[MO])
            attention(C, ptl, "mb%d" % h, qT, kT, QK, vb, VB, 128, T0b, T1b, TBK, exp_transform(C, scale), on_O, extra=extra)
        junk = sb("junk", [128, 1024]); JK = S.buf("mb_junk")
        yo = [sb("yo%d" % i, [128, 1024]) for i in range(2)]; YO = S.bufs("mb_yo", 2)
        for qt in range(16):
            ss = stt[:, 16 + qt:17 + qt]; sd = stt[:, 32 + qt:33 + qt]; rs = stt[:, 48 + qt:49 + qt]
            S.op("act", lambda: nc.scalar.activation(out=junk[:], in_=mo[:, qt, :], func=AF.Square, accum_out=ss), reads=[MO], writes=[JK, STT])
            rstd_ops(C, ss, sd, rs, 1024, STT)
            y = yo[qt % 2]; Y = YO[qt % 2]
            S.op("dve", lambda: nc.vector.scalar_tensor_tensor(out=y[:], in0=mo[:, qt, :], scalar=rs, in1=monw[:], op0=ALU.mult, op1=ALU.mult),
                 reads=[MO, STT, MW], writes=[Y])
            S.dma("sp", mixed_d[qt * 128:(qt + 1) * 128, 2048:3072], y[:], reads=[Y])
        S.barrier()


def ssd_mixer(C, projF, projT, negmask_d, convw_d, convb_d, dtb_d, alog_d, dsk_d, nw_d, acs_d, mixed_d):
    nc, S = C.nc, C.S
    with ExitStack() as st:
        sb = lambda n, s, d=F32: st.enter_context(_sbt(nc, "sd_" + n, s, d))
        negm = sb("negm", [128, 128]); NM = S.buf("sd_negm")
        S.dma("sp", negm[:], negmask_d[:, :], writes=[NM])
        cw = sb("cw", [128, 16, 4]); cb = sb("cb", [128, 16]); CW = S.buf("sd_cw")
        with nc.allow_non_contiguous_dma(reason="small param transposes"):
            for j in range(4):
                S.dma("sp", cw[:, :, j], convw_d[j, 0, :].rearrange("(c p) -> p c", p=128), writes=[CW])
            S.dma("sp", cb[:], convb_d.rearrange("(c p) -> p c", p=128), writes=[CW])
        dsk = sb("dsk", [128, 16]); DSK = S.buf("sd_dsk")
        S.dma("sp", dsk[:], dsk_d.partition_broadcast(128), writes=[DSK])
        nwb = sb("nwb", [128, 1024]); NWB = S.buf("sd_nwb")
        S.dma("sp", nwb[:], nw_d.partition_broadcast(128), writes=[NWB])
        xtm = sb("xtm", [128, 16, 1024]); XTM = S.buf("sd_xtm")
        bc = sb("bc", [128, 8, 2048], BF16); BCK = S.buf("sd_bc")
        vbx = sb("vbx", [128, 16, 16, 64], BF16); VBX = S.buf("sd_vbx")
        dtT = sb("dtT", [128, 16, 16]); nacT = sb("nacT", [128, 16, 16]); DTT = S.buf("sd_dtT")
        with ExitStack() as st2:
            sb2 = lambda n, s, d=F32: st2.enter_context(_sbt(nc, "sd2_" + n, s, d))
            dtr = sb2("dtr", [16, 2048]); dtv = sb2("dtv", [16, 2048]); av = sb2("av", [16, 2048]); ones = sb2("ones", [16, 2048])
            acu = sb2("acu", [16, 2048]); sm = sb2("sm", [16, 4]); DTK = S.buf("sd_dtk"); SM = S.buf("sd_sm")
            with nc.allow_non_contiguous_dma(reason="small param columns"):
                S.dma("sp", sm[:, 0:1], dtb_d.rearrange("(h o) -> h o", o=1), writes=[SM])
                S.dma("sp", sm[:, 1:2], alog_d.rearrange("(h o) -> h o", o=1), writes=[SM])
            S.op("act", lambda: nc.scalar.activation(out=sm[:, 2:3], in_=sm[:, 1:2], func=AF.Exp), reads=[SM], writes=[SM])
            S.op("dve", lambda: nc.vector.tensor_scalar(out=sm[:, 3:4], in0=sm[:, 2:3], scalar1=-1.0, scalar2=None, op0=ALU.mult),
                 reads=[SM], writes=[SM])
            S.dma("sp", dtr[:], projF[10240:10256, :], writes=[DTK])
            S.op("act", lambda: nc.scalar.activation(out=dtv[:], in_=dtr[:], func=AF.Exp, bias=sm[:, 0:1], scale=1.0),
                 reads=[DTK, SM], writes=[DTK])
            S.op("act", lambda: nc.scalar.activation(out=dtv[:], in_=dtv[:], func=AF.Ln, bias=1.0, scale=1.0), reads=[DTK], writes=[DTK])
            S.op("dve", lambda: nc.vector.tensor_scalar(out=av[:], in0=dtv[:], scalar1=sm[:, 3:4], scalar2=None, op0=ALU.mult),
                 reads=[DTK, SM], writes=[DTK])
            S.op("pool", lambda: nc.gpsimd.memset(ones[:], 1.0), writes=[DTK])
            S.op("dve", lambda: nc.vector.tensor_tensor_scan(out=acu[:], data0=ones[:], data1=av[:], initial=0.0, op0=ALU.mult, op1=ALU.add),
                 reads=[DTK], writes=[DTK])
            S.dma("sp", acs_d[:, :], acu[:], reads=[DTK], writes=[DTK])
            for tt in range(16):
                S.op("pe", lambda: nc.tensor.transpose(C.bank(6)[:, 0:16], dtv[:, tt * 128:(tt + 1) * 128], C.identf[0:16, 0:16]),
                     reads=[DTK, C.IDT], writes=[C.PS[6]])
                S.op("dve", lambda: nc.vector.tensor_copy(out=dtT[:, tt, :], in_=C.bank(6)[:, 0:16]), reads=[C.PS[6]], writes=[DTT])
                S.op("pe", lambda: nc.tensor.transpose(C.bank(7)[:, 0:16], acu[:, tt * 128:(tt + 1) * 128], C.identf[0:16, 0:16]),
                     reads=[DTK, C.IDT], writes=[C.PS[7]])
                S.op("act", lambda: nc.scalar.mul(out=nacT[:, tt, :], in_=C.bank(7)[:, 0:16], mul=-1.0), reads=[C.PS[7]], writes=[DTT])
            S.barrier()
        with ExitStack() as st2:
            sb2 = lambda n, s, d=F32: st2.enter_context(_sbt(nc, "sd3_" + n, s, d))
            xin = [sb2("xin%d" % i, [128, 2048]) for i in range(2)]; XIN = S.bufs("sd_xin", 2)
            acc = [sb2("acc%d" % i, [128, 2048]) for i in range(2)]; ACC = S.bufs("sd_acc", 2)
            for c in range(16):
                i = c % 2
                X = xin[i]; A = acc[i]
                S.dma("sp", X[:], projF[8192 + c * 128:8192 + (c + 1) * 128, :], writes=[XIN[i]])
                S.op("dve", lambda: nc.vector.tensor_scalar(out=A[:], in0=X[:], scalar1=cw[:, c, 3:4], scalar2=cb[:, c:c + 1],
                                                            op0=ALU.mult, op1=ALU.add), reads=[XIN[i], CW], writes=[ACC[i]])
                for sft in (1, 2, 3):
                    S.op("dve", lambda: nc.vector.scalar_tensor_tensor(out=A[:, sft:2048], in0=X[:, 0:2048 - sft], scalar=cw[:, c, 3 - sft:4 - sft],
                                                                       in1=A[:, sft:2048], op0=ALU.mult, op1=ALU.add),
                         reads=[XIN[i], CW, ACC[i]], writes=[ACC[i]])
                if c < 8:
                    S.op("act", lambda: nc.scalar.activation(out=A[:], in_=A[:], func=AF.Silu), reads=[ACC[i]], writes=[ACC[i]])
                    for t4 in range(4):
                        b = 6 + (t4 % 2)
                        fns = []
                        for k in range(4):
                            tt = t4 * 4 + k
                            fns.append(lambda k=k, tt=tt: nc.tensor.transpose(C.bank(b)[:, k * 128:(k + 1) * 128], A[:, tt * 128:(tt + 1) * 128], C.identf[:]))
                        S.group("pe", fns, reads=[ACC[i], C.IDT], writes=[C.PS[b]])
                        S.op("dve", lambda: nc.vector.tensor_copy(out=xtm[:, t4 * 4:(t4 + 1) * 4, c * 128:(c + 1) * 128],
                                                                  in_=C.bank(b).rearrange("p (k q) -> p k q", k=4)), reads=[C.PS[b]], writes=[XTM])
                else:
                    S.op("act", lambda: nc.scalar.activation(out=bc[:, c - 8, :], in_=A[:], func=AF.Silu), reads=[ACC[i]], writes=[BCK])
            S.barrier()
        for tt in range(16):
            eng = "dve" if tt % 2 == 0 else "pool"
            f = (nc.vector if eng == "dve" else nc.gpsimd)
            S.op(eng, lambda: f.tensor_tensor(out=vbx[:, tt, :, :], in0=xtm[:, tt, :].rearrange("p (h d) -> p h d", d=64),
                                              in1=dtT[:, tt, :].unsqueeze(2).to_broadcast([128, 16, 64]), op=ALU.mult),
                 reads=[XTM, DTT], writes=[VBX])
        acb = [sb("acb%d" % i, [128, 2048]) for i in range(2)]; ACB = S.bufs("sd_acb", 2)
        et = [sb("et%d" % i, [128, 512]) for i in range(3)]; ET = S.bufs("sd_et", 3)
        dg = sb("dg", [128, 128]); DG = S.buf("sd_dg")
        ptl = ([sb("pt%d" % i, [128, 512], BF16) for i in range(4)], S.bufs("sd_pt", 4))
        cnt = [0]
        for h in range(16):
            g = h // 4
            ab = acb[h % 2]; AB = ACB[h % 2]
            S.dma("sp", ab[:], acs_d[h, :].partition_broadcast(128), writes=[AB])

            def transform(kt, qb, c0, pap, oap, reads, writes, h=h, ab=ab, AB=AB):
                e = et[cnt[0] % 3]; E = ET[cnt[0] % 3]
                cnt[0] += 1
                nb = nacT[:, kt, h:h + 1]
                q0 = qb * 512
                lo = c0
                if kt >= 4 * qb:
                    S.op("dve", lambda: nc.vector.tensor_tensor(out=dg[:], in0=ab[:, q0 + c0:q0 + c0 + 128], in1=negm[:], op=ALU.add),
                         reads=[AB, NM], writes=[DG])
                    S.op("act", lambda: nc.scalar.activation(out=e[:, c0:c0 + 128], in_=dg[:], func=AF.Exp, bias=nb, scale=1.0),
                         reads=[DG, DTT], writes=[E])
                    lo = c0 + 128
                if lo < 512:
                    S.op("act", lambda: nc.scalar.activation(out=e[:, lo:512], in_=ab[:, q0 + lo:q0 + 512], func=AF.Exp, bias=nb, scale=1.0),
                         reads=[AB, DTT], writes=[E])
                S.op("dve", lambda: nc.vector.tensor_tensor(out=oap, in0=pap, in1=e[:, c0:512], op=ALU.mult), reads=reads + [E], writes=writes)

            def on_O(qt, oap, otok, h=h):
                S.op("dve", lambda: nc.vector.scalar_tensor_tensor(out=xtm[:, qt, h * 64:(h + 1) * 64], in0=xtm[:, qt, h * 64:(h + 1) * 64],
                                                                   scalar=dsk[:, h:h + 1], in1=oap, op0=ALU.mult, op1=ALU.add),
                     reads=[otok, XTM, DSK], writes=[XTM])
            attention(C, ptl, "sd%d" % h, bc[:, 4 + g, :], bc[:, g, :], BCK, vbx[:, :, h, :], VBX, 64, None, None, None, transform, on_O)
        zt = [sb("zt%d" % i, [128, 1024]) for i in range(2)]; ZT = S.bufs("sd_zt", 2)
        junk = sb("junk", [128, 256]); JK = S.buf("sd_junk")
        stt = sb("stt", [128, 16 * 12]); STT = S.buf("sd_stt")
        for qt in range(16):
            z = zt[qt % 2]; Z = ZT[qt % 2]
            S.dma("sp", z[:], projT[qt * 128:(qt + 1) * 128, 7168:8192], writes=[Z])
            S.op("act", lambda: nc.scalar.activation(out=z[:], in_=z[:], func=AF.Silu), reads=[Z], writes=[Z])
            S.op("dve", lambda: nc.vector.tensor_tensor(out=z[:], in0=z[:], in1=xtm[:, qt, :], op=ALU.mult), reads=[Z, XTM], writes=[Z])
            for g in range(4):
                c = qt * 12 + g * 3
                ss = stt[:, c:c + 1]; sd = stt[:, c + 1:c + 2]; rs = stt[:, c + 2:c + 3]
                S.op("act", lambda: nc.scalar.activation(out=junk[:], in_=z[:, g * 256:(g + 1) * 256], func=AF.Square, accum_out=ss),
                     reads=[Z], writes=[JK, STT])
                rstd_ops(C, ss, sd, rs, 256, STT)
                S.op("dve", lambda: nc.vector.scalar_tensor_tensor(out=z[:, g * 256:(g + 1) * 256], in0=z[:, g * 256:(g + 1) * 256], scalar=rs,
                                                                   in1=nwb[:, g * 256:(g + 1) * 256], op0=ALU.mult, op1=ALU.mult),
                     reads=[Z, STT, NWB], writes=[Z])
            S.dma("sp", mixed_d[qt * 128:(qt + 1) * 128, 3072:4096], z[:], reads=[Z])
        S.barrier()


TWO_PI = 6.283185307179586


def s5_mixer(C, projF, prm, iota_d, m2_d, m3_d, m4_d, gtm_d, mixed_d, gTd=None, tabd=None):
    nc, S = C.nc, C.S
    V, G = nc.vector, nc.gpsimd

    def tt(eng, out, in0, in1, op, reads, writes):
        f = V if eng == "dve" else G
        return S.op(eng, lambda: f.tensor_tensor(out=out, in0=in0, in1=in1, op=op), reads=reads, writes=writes)

    def ts(eng, out, in0, s1, op0, reads, writes, s2=None, op1=None):
        f = V if eng == "dve" else G
        if op1 is None:
            return S.op(eng, lambda: f.tensor_scalar(out=out, in0=in0, scalar1=s1, scalar2=None, op0=op0), reads=reads, writes=writes)
        return S.op(eng, lambda: f.tensor_scalar(out=out, in0=in0, scalar1=s1, scalar2=s2, op0=op0, op1=op1), reads=reads, writes=writes)

    def wrap_half(eng, r, m, RK):
        ts(eng, m, r, 0.5, ALU.is_gt, [RK], [RK])
        tt(eng, r, r, m, ALU.subtract, [RK], [RK])
        ts(eng, m, r, -0.5, ALU.is_lt, [RK], [RK])
        tt(eng, r, r, m, ALU.add, [RK], [RK])

    def frac_reduce(eng, kf, ki, r, m, RK):
        S.op("dve", lambda: V.tensor_copy(out=ki, in_=kf), reads=[RK], writes=[RK])
        S.op("dve", lambda: V.tensor_copy(out=r, in_=ki), reads=[RK], writes=[RK])
        tt(eng, r, kf, r, ALU.subtract, [RK], [RK])
        wrap_half(eng, r, m, RK)

    SINS = TWO_PI * (1.0 - 2e-6)
    with ExitStack() as st:
        stp = ExitStack()
        GT = S.buf("s5_gT")
        sb = lambda n, s, d=F32: stp.enter_context(_sbt(nc, "s5_" + n, s, d))
        mag = sb("mag", [128, 32]); thn = sb("thn", [128, 32]); dcol = sb("dcol", [128, 8]); iot = sb("iot", [128, 2048])
        LTzr = [sb("LTzr%d" % k, [128, 8, 128], BF16) for k in range(4)]; LTzi = [sb("LTzi%d" % k, [128, 8, 128], BF16) for k in range(4)]
        Lzr = [sb("Lzr%d" % k, [128, 8, 128], BF16) for k in range(4)]; Lzi = [sb("Lzi%d" % k, [128, 8, 128], BF16) for k in range(4)]
        sts = ExitStack()
        sb = lambda n, s, d=F32: sts.enter_context(_sbt(nc, "s5_" + n, s, d))
        P = S.buf("s5_prm")
        lr = sb("lr", [128, 32]); li = sb("li", [128, 32]); ldt = sb("ldt", [128, 32])
        with nc.allow_non_contiguous_dma(reason="small param layouts"):
            S.dma("sp", lr[:], prm["lam_re"].rearrange("g p -> (g p)").rearrange("(j q) -> q j", q=128), writes=[P])
            S.dma("sp", li[:], prm["lam_im"].rearrange("g p -> (g p)").rearrange("(j q) -> q j", q=128), writes=[P])
            for g2 in range(2):
                S.dma("sp", ldt[g2 * 64:(g2 + 1) * 64, :], prm["log_dt"].rearrange("(j g) -> g j", g=2)[g2, :].partition_broadcast(64), writes=[P])
        dtv = sb("dtv", [128, 32])
        ki32 = sb("ki32", [128, 32], I32); r0 = sb("r0", [128, 32]); m0 = sb("m0", [128, 32]); rc0 = sb("rc0", [128, 32])
        sn0 = sb("sn0", [128, 32]); cs0 = sb("cs0", [128, 32])
        S.op("act", lambda: nc.scalar.activation(out=dtv[:], in_=ldt[:], func=AF.Exp), reads=[P], writes=[P])
        tt("dve", mag[:], lr[:], dtv[:], ALU.mult, [P], [P])
        S.op("act", lambda: nc.scalar.activation(out=mag[:], in_=mag[:], func=AF.Exp), reads=[P], writes=[P])
        tt("dve", thn[:], li[:], dtv[:], ALU.mult, [P], [P])
        ts("dve", thn[:], thn[:], 1.0 / TWO_PI, ALU.mult, [P], [P])
        frac_reduce("dve", thn[:], ki32[:], r0[:], m0[:], P)
        S.op("act", lambda: nc.scalar.activation(out=sn0[:], in_=r0[:], func=AF.Sin, scale=SINS), reads=[P], writes=[P])
        ts("dve", rc0[:], r0[:], 0.25, ALU.add, [P], [P])
        ts("dve", m0[:], rc0[:], 0.5, ALU.is_gt, [P], [P])
        tt("dve", rc0[:], rc0[:], m0[:], ALU.subtract, [P], [P])
        S.op("act", lambda: nc.scalar.activation(out=cs0[:], in_=rc0[:], func=AF.Sin, scale=SINS), reads=[P], writes=[P])
        abr = sb("abr", [128, 32]); abi = sb("abi", [128, 32]); den = sb("den", [128, 32]); t0 = sb("t0", [128, 32]); t1 = sb("t1", [128, 32])
        fre = sb("fre", [128, 32]); fim = sb("fim", [128, 32])
        tt("dve", abr[:], mag[:], cs0[:], ALU.mult, [P], [P])
        ts("dve", abr[:], abr[:], -1.0, ALU.add, [P], [P])
        tt("dve", abi[:], mag[:], sn0[:], ALU.mult, [P], [P])
        tt("dve", den[:], lr[:], lr[:], ALU.mult, [P], [P])
        tt("dve", t0[:], li[:], li[:], ALU.mult, [P], [P])
        tt("dve", den[:], den[:], t0[:], ALU.add, [P], [P])
        S.op("dve", lambda: V.reciprocal(out=den[:], in_=den[:]), reads=[P], writes=[P])
        tt("dve", t0[:], abr[:], lr[:], ALU.mult, [P], [P])
        tt("dve", t1[:], abi[:], li[:], ALU.mult, [P], [P])
        tt("dve", fre[:], t0[:], t1[:], ALU.add, [P], [P])
        tt("dve", fre[:], fre[:], den[:], ALU.mult, [P], [P])
        tt("dve", t0[:], abi[:], lr[:], ALU.mult, [P], [P])
        tt("dve", t1[:], abr[:], li[:], ALU.mult, [P], [P])
        tt("dve", fim[:], t0[:], t1[:], ALU.subtract, [P], [P])
        tt("dve", fim[:], fim[:], den[:], ALU.mult, [P], [P])
        br = sb("br", [128, 32, 16]); bi = sb("bi", [128, 32, 16]); bbr = sb("bbr", [128, 32, 16]); bbi = sb("bbi", [128, 32, 16])
        tb = sb("tb", [128, 32, 16])
        S.dma("sp", br[:], prm["b_re"].rearrange("g p h -> (g p h)").rearrange("(j q h) -> q j h", q=128, h=16), writes=[P])
        S.dma("sp", bi[:], prm["b_im"].rearrange("g p h -> (g p h)").rearrange("(j q h) -> q j h", q=128, h=16), writes=[P])
        fre_b = fre[:].unsqueeze(2).to_broadcast([128, 32, 16]); fim_b = fim[:].unsqueeze(2).to_broadcast([128, 32, 16])
        tt("dve", bbr[:], br[:], fre_b, ALU.mult, [P], [P])
        tt("dve", tb[:], bi[:], fim_b, ALU.mult, [P], [P])
        tt("dve", bbr[:], bbr[:], tb[:], ALU.subtract, [P], [P])
        tt("dve", bbi[:], bi[:], fre_b, ALU.mult, [P], [P])
        tt("dve", tb[:], br[:], fim_b, ALU.mult, [P], [P])
        tt("dve", bbi[:], bbi[:], tb[:], ALU.add, [P], [P])
        m2 = sb("m2", [128, 2]); m3 = sb("m3", [128, 2])
        S.dma("sp", m2[:], m2_d[:, :], writes=[P])
        S.dma("sp", m3[:], m3_d[:, :], writes=[P])
        LTr = sb("LTr", [128, 8, 128], BF16); LTi = sb("LTi", [128, 8, 128], BF16); LTK = S.buf("s5_LT")
        bx = sb("bx", [128, 4, 2, 16], BF16); BX = S.buf("s5_bx")
        for (src, dst) in ((bbr, LTr), (bbi, LTi)):
            for c in range(8):
                S.op("dve", lambda: V.tensor_tensor(out=bx[:], in0=src[:, 4 * c:4 * c + 4, :].unsqueeze(2).to_broadcast([128, 4, 2, 16]),
                                                    in1=m2[:].unsqueeze(1).unsqueeze(3).to_broadcast([128, 4, 2, 16]), op=ALU.mult),
                     reads=[P], writes=[BX])
                pb = C.bank_bf(7)
                S.op("pe", lambda: nc.tensor.transpose(pb[:, 0:128], bx[:].rearrange("p a b c -> p (a b c)"), C.ident[:]),
                     reads=[BX, C.IDT], writes=[C.PS[7]])
                S.op("act", lambda: nc.scalar.copy(out=dst[:, c, :], in_=pb[:, 0:128]), reads=[C.PS[7]], writes=[LTK])
        crn = sb("crn", [128, 8, 64]); cin = sb("cin", [128, 8, 64])
        S.dma("sp", crn[:], prm["c_re"].rearrange("g h p -> (g h p)").rearrange("(c q p) -> q c p", q=128, p=64), writes=[P])
        S.dma("sp", cin[:], prm["c_im"].rearrange("g h p -> (g h p)").rearrange("(c q p) -> q c p", q=128, p=64), writes=[P])
        Lr = sb("Lr", [128, 8, 128], BF16); Li = sb("Li", [128, 8, 128], BF16); LCK = S.buf("s5_LC")
        cx = sb("cx", [128, 2, 64], BF16); CX = S.buf("s5_cx")
        for (src, dst, sgn) in ((crn, Lr, 1.0), (cin, Li, -1.0)):
            for c in range(8):
                S.op("dve", lambda: V.tensor_tensor(out=cx[:], in0=src[:, c, :].unsqueeze(1).to_broadcast([128, 2, 64]),
                                                    in1=m3[:].unsqueeze(2).to_broadcast([128, 2, 64]), op=ALU.mult), reads=[P], writes=[CX])
                pb = C.bank_bf(7)
                S.op("pe", lambda: nc.tensor.transpose(pb[:, 0:128], cx[:].rearrange("p a b -> p (a b)"), C.ident[:]),
                     reads=[CX, C.IDT], writes=[C.PS[7]])
                S.op("act", lambda: nc.scalar.mul(out=dst[:, c, :], in_=pb[:, 0:128], mul=sgn), reads=[C.PS[7]], writes=[LCK])
        m4 = sb("m4", [128, 4])
        S.dma("sp", m4[:], m4_d[:, :], writes=[P])
        for k in range(4):
            for (src, dst) in ((LTr, LTzr[k]), (LTi, LTzi[k])):
                S.op("dve", lambda: V.tensor_scalar(out=dst[:], in0=src[:], scalar1=m4[:, k:k + 1], scalar2=None, op0=ALU.mult), reads=[LTK, P], writes=[LTK])
            for (src, dst) in ((Lr, Lzr[k]), (Li, Lzi[k])):
                S.op("pool", lambda: G.memset(dst[:], 0.0), writes=[LCK])
                S.op("dve", lambda: V.tensor_copy(out=dst[:, :, 32 * k:32 * k + 32], in_=src[:, :, 32 * k:32 * k + 32]), reads=[LCK], writes=[LCK])
        with nc.allow_non_contiguous_dma(reason="small"):
            S.dma("sp", dcol[:], prm["d"].rearrange("g h -> (g h)").rearrange("(c q) -> q c", q=128), writes=[P])
        IOT = S.buf("s5_iot")
        S.dma("sp", iot[:], iota_d.partition_broadcast(128), writes=[IOT])
        S.barrier()
        sts.close()
        with ExitStack() as st2:
            sb2 = lambda n, s, d=F32: st2.enter_context(_sbt(nc, "s5b_" + n, s, d))
            ufl = [sb2("uf%d" % i, [128, 2048]) for i in range(2)]; ubl = [sb2("ub%d" % i, [128, 2048], BF16) for i in range(2)]; UFL = S.bufs("s5_uf", 2)
            cosT = sb2("cosT", [128, 2048]); sinT = sb2("sinT", [128, 2048]); TBL = S.buf("s5_tbl")
            x1 = sb2("x1", [128, 2048]); x2 = sb2("x2", [128, 2048]); x3 = sb2("x3", [128, 2048]); x4 = sb2("x4", [128, 2048])
            X1, X2, X3, X4 = S.bufs("s5_x", 4)
            bre = sb2("bre", [128, 2048]); bim = sb2("bim", [128, 2048]); BRE = S.buf("s5_bre"); BIM = S.buf("s5_bim")
            sre = sb2("sre", [128, 4, 2048], BF16); sim = sb2("sim", [128, 4, 2048], BF16); SK = S.buf("s5_s")
            yT = sb2("yT", [128, 2048]); YK = S.buf("s5_yT")
            gx = sb2("gx", [128, 2048]); XK = S.buf("s5_gx")
            gb = sb2("gb", [128, 2048], BF16); GB = S.buf("s5_gb")
            gst = Stager(C, st2, "s5_gst", 1)
            HALF_PI = 1.5707963267948966

            cosL = [cosT, sb2("cosT1", [128, 2048])]; sinL = [sinT, sb2("sinT1", [128, 2048])]
            TBLS = [TBL, S.buf("s5_tbl1")]

            def A_load(j):
                S.dma("sp", cosL[j % 2][:], tabd[j, 0, :, :], writes=[TBLS[j % 2]])
                S.dma("sp", sinL[j % 2][:], tabd[j, 1, :, :], writes=[TBLS[j % 2]])

            def A_dve(j):
                pass

            def A_act(j):
                if j + 1 < 32:
                    A_load(j + 1)

            A_load(0)
            A_load(1)
            for c in range(8):
                uf = ufl[c % 2]; ub = ubl[c % 2]; UF = UFL[c % 2]
                S.dma("sp", uf[:], projF[c * 128:(c + 1) * 128, :], writes=[UF])
                S.op("act", lambda: nc.scalar.copy(out=ub[:], in_=uf[:]), reads=[UF], writes=[UF])
                for jj in range(4):
                    j = 4 * c + jj
                    fns = []
                    for tb_ in range(4):
                        for (LT, b0) in ((LTzr[jj], 0), (LTzi[jj], 4)):
                            fns.append(lambda tb_=tb_, LT=LT, b0=b0: nc.tensor.matmul(
                                C.bank(b0 + tb_), LT[:, c, :], ub[:, tb_ * 512:(tb_ + 1) * 512],
                                start=True, stop=True))
                    S.group("pe", fns, reads=[LTK, UF], writes=C.PS)
                    pre = C.ps[:, 0:2048]; pim = C.ps[:, 2048:4096]
                    cosT = cosL[j % 2]; sinT = sinL[j % 2]; TBL = TBLS[j % 2]
                    tt("dve", x1[:], pre, cosT[:], ALU.mult, C.PS + [TBL], [X1])
                    tt("dve", x2[:], pim, sinT[:], ALU.mult, C.PS + [TBL], [X2])
                    tt("dve", x3[:], pim, cosT[:], ALU.mult, C.PS + [TBL], [X3])
                    tt("dve", x4[:], pre, sinT[:], ALU.mult, C.PS + [TBL], [X4])
                    tt("pool", bre[:], x1[:], x2[:], ALU.subtract, [X1, X2], [BRE])
                    tt("pool", bim[:], x3[:], x4[:], ALU.add, [X3, X4], [BIM])
                    if j + 1 < 32:
                        A_dve(j + 1)
                    magb = mag[:, j:j + 1].to_broadcast([128, 2048])
                    S.op("dve", lambda: V.tensor_tensor_scan(out=bre[:], data0=magb, data1=bre[:], initial=0.0, op0=ALU.mult, op1=ALU.add),
                         reads=[BRE, P], writes=[BRE])
                    S.op("dve", lambda: V.tensor_tensor_scan(out=bim[:], data0=magb, data1=bim[:], initial=0.0, op0=ALU.mult, op1=ALU.add),
                         reads=[BIM, P], writes=[BIM])
                    tt("dve", x1[:], bre[:], cosT[:], ALU.mult, [BRE, TBL], [X1])
                    tt("pool", x4[:], bre[:], sinT[:], ALU.mult, [BRE, TBL], [X4])
                    tt("dve", x3[:], bim[:], cosT[:], ALU.mult, [BIM, TBL], [X3])
                    tt("pool", x2[:], bim[:], sinT[:], ALU.mult, [BIM, TBL], [X2])
                    tt("dve", sre[:, jj, :], x1[:], x2[:], ALU.add, [X1, X2], [SK])
                    tt("pool", sim[:, jj, :], x3[:], x4[:], ALU.subtract, [X3, X4], [SK])
                    if j + 1 < 32:
                        A_act(j + 1)
                for tb_ in range(4):
                    b = tb_ % 2
                    fns = []
                    for jj in range(4):
                        fns.append(lambda jj=jj: nc.tensor.matmul(C.bank(b), Lzr[jj][:, c, :],
                                                                  sre[:, jj, tb_ * 512:(tb_ + 1) * 512], start=(jj == 0), stop=False))
                        fns.append(lambda jj=jj: nc.tensor.matmul(C.bank(b), Lzi[jj][:, c, :],
                                                                  sim[:, jj, tb_ * 512:(tb_ + 1) * 512], start=False, stop=(jj == 3)))
                    S.group("pe", fns, reads=[LCK, SK], writes=[C.PS[b]])
                    S.op("dve", lambda: V.scalar_tensor_tensor(out=yT[:, tb_ * 512:(tb_ + 1) * 512], in0=uf[:, tb_ * 512:(tb_ + 1) * 512],
                                                               scalar=dcol[:, c:c + 1], in1=C.bank(b), op0=ALU.mult, op1=ALU.add),
                         reads=[UF, P, C.PS[b]], writes=[YK])
                tt("pool", gx[:], yT[:], yT[:], ALU.mult, [YK], [XK])
                ts("pool", gx[:], gx[:], 0.044715, ALU.mult, [XK], [XK], s2=1.0, op1=ALU.add)
                tt("pool", gx[:], gx[:], yT[:], ALU.mult, [XK, YK], [XK])
                S.op("act", lambda: nc.scalar.activation(out=gx[:], in_=gx[:], func=AF.Sigmoid, scale=1.5957691216), reads=[XK], writes=[XK])
                tt("pool", yT[:], yT[:], gx[:], ALU.mult, [XK, YK], [YK])
                S.op("act", lambda: nc.scalar.copy(out=gb[:], in_=yT[:]), reads=[YK], writes=[GB])
                S.dma("pool", gTd[c * 128:(c + 1) * 128, :], gb[:], reads=[GB])
                for t4 in range(4):
                    b = 2 + (t4 % 2)
                    fns = []
                    for k in range(4):
                        tq = t4 * 4 + k
                        fns.append(lambda k=k, tq=tq: nc.tensor.transpose(C.bank(b)[:, k * 128:(k + 1) * 128], yT[:, tq * 128:(tq + 1) * 128], C.identf[:]))
                    S.group("pe", fns, reads=[YK, C.IDT], writes=[C.PS[b]])
                    g_, gk, eng = gst.next()
                    copy_op(C, eng, g_[:], C.bank(b), [C.PS[b]], [gk])
                    S.dma("sp", gtm_d[t4 * 512:(t4 + 1) * 512, c * 128:(c + 1) * 128].rearrange("(k p) n -> p k n", p=128),
                          g_[:].rearrange("p (k n) -> p k n", k=4), reads=[gk])
            S.barrier()
        stp.close()
        with ExitStack() as st2:
            sb2 = lambda n, s, d=F32: st2.enter_context(_sbt(nc, "s5c_" + n, s, d))
            otm = sb2("otm", [128, 16, 1024]); OT = S.buf("s5_otm")
            onw = sb2("onw", [128, 1024]); ONW = S.buf("s5_onw")
            S.dma("sp", onw[:], prm["onw"].partition_broadcast(128), writes=[ONW])
            sg = Stager(C, st2, "s5_sg", 2)
            gi = Stager(C, st2, "s5_gi", 2)
            for half in range(2):
                def evac(mode, c0, t_, pap, ptok, half=half):
                    tq = half * 8 + t_
                    s_, sk, _ = sg.next()
                    g_, gk, _ = gi.next()
                    S.dma("sp", g_[:], gtm_d[tq * 128:(tq + 1) * 128, c0:c0 + 512], writes=[gk])
                    S.op("act", lambda: nc.scalar.activation(out=s_[:], in_=pap, func=AF.Sigmoid), reads=[ptok], writes=[sk])
                    S.op("dve", lambda: V.tensor_tensor(out=otm[:, tq, c0:c0 + 512], in0=s_[:], in1=g_[:], op=ALU.mult), reads=[sk, gk], writes=[OT])
                gTh = sb2("gTh%d" % half, [128, 8, 1024], BF16)
                S.dma("sp", gTh[:], gTd.rearrange("(c p) t -> p c t", p=128)[:, :, half * 1024:(half + 1) * 1024], writes=[GT])
                dense(C, gTh, GT, 8, 1024, prm["w_glu"], [(0, 512, "tm"), (512, 512, "tm")], evac, name="glu%d" % half)
            junk = sb2("junk", [128, 1024]); JK = S.buf("s5_junk")
            stt = sb2("stt", [128, 64]); STT = S.buf("s5_stt")
            yo = [sb2("yo%d" % i, [128, 1024]) for i in range(2)]; YO = S.bufs("s5_yo", 2)
            for qt in range(16):
                ss = stt[:, qt:qt + 1]; sd = stt[:, 16 + qt:17 + qt]; rs = stt[:, 32 + qt:33 + qt]
                S.op("act", lambda: nc.scalar.activation(out=junk[:], in_=otm[:, qt, :], func=AF.Square, accum_out=ss), reads=[OT], writes=[JK, STT])
                rstd_ops(C, ss, sd, rs, 1024, STT)
                y = yo[qt % 2]; Y = YO[qt % 2]
                S.op("dve", lambda: V.scalar_tensor_tensor(out=y[:], in0=otm[:, qt, :], scalar=rs, in1=onw[:], op0=ALU.mult, op1=ALU.mult),
                     reads=[OT, STT, ONW], writes=[Y])
                S.dma("sp", mixed_d[qt * 128:(qt + 1) * 128, 0:1024], y[:], reads=[Y])
            S.barrier()


class S5TableGen:
    def __init__(self, C, st, lam_im_d, log_dt_d, iota_d, tabd):
        nc, S = C.nc, C.S
        self.C = C
        sb = lambda n, s, d=F32: st.enter_context(_sbt(nc, "tg_" + n, s, d))
        self.thn = sb("thn", [128, 32]); li = sb("li", [128, 32]); ldt = sb("ldt", [128, 32])
        self.P = S.buf("tg_p")
        with nc.allow_non_contiguous_dma(reason="small param layouts"):
            S.dma("sp", li[:], lam_im_d.rearrange("g p -> (g p)").rearrange("(j q) -> q j", q=128), writes=[self.P])
            for g2 in range(2):
                S.dma("sp", ldt[g2 * 64:(g2 + 1) * 64, :], log_dt_d.rearrange("(j g) -> g j", g=2)[g2, :].partition_broadcast(64), writes=[self.P])
        S.op("act", lambda: nc.scalar.activation(out=ldt[:], in_=ldt[:], func=AF.Exp), reads=[self.P], writes=[self.P])
        S.op("dve", lambda: nc.vector.tensor_tensor(out=self.thn[:], in0=li[:], in1=ldt[:], op=ALU.mult), reads=[self.P], writes=[self.P])
        S.op("dve", lambda: nc.vector.tensor_scalar(out=self.thn[:], in0=self.thn[:], scalar1=1.0 / TWO_PI, scalar2=None, op0=ALU.mult),
             reads=[self.P], writes=[self.P])
        self.iot = sb("iot", [128, 2048]); self.IOT = S.buf("tg_iot")
        S.dma("sp", self.iot[:], iota_d.partition_broadcast(128), writes=[self.IOT])
        self.kf = sb("kf", [128, 2048]); self.ki = sb("ki", [128, 2048], I32); self.rb = sb("rb", [128, 2048])
        self.RK = S.buf("tg_rk"); self.RB = S.buf("tg_rb")
        self.cosb = [sb("cos%d" % i, [128, 2048]) for i in range(2)]; self.sinb = [sb("sin%d" % i, [128, 2048]) for i in range(2)]
        self.CB = S.bufs("tg_cb", 2); self.SB = S.bufs("tg_sb", 2)
        self.tabd = tabd
        self.gen = self._run()
        self.done = False

    def _run(self):
        C = self.C
        nc, S = C.nc, C.S
        V = nc.vector
        SINS = TWO_PI * (1.0 - 2e-6)
        HALF_PI = 1.5707963267948966
        kf, ki, rb = self.kf, self.ki, self.rb
        RK, RB = self.RK, self.RB
        for j in range(32):
            b = j % 2
            S.op("dve", lambda: V.tensor_scalar(out=kf[:], in0=self.iot[:], scalar1=self.thn[:, j:j + 1], scalar2=8.0, op0=ALU.mult, op1=ALU.add),
                 reads=[self.IOT, self.P], writes=[RK])
            yield
            S.op("dve", lambda: V.tensor_copy(out=ki[:], in_=kf[:]), reads=[RK], writes=[RK])
            yield
            S.op("dve", lambda: V.tensor_tensor(out=rb[:], in0=kf[:], in1=ki[:], op=ALU.subtract), reads=[RK], writes=[RB])
            yield
            S.op("dve", lambda: V.scalar_tensor_tensor(out=kf[:], in0=rb[:], scalar=0.5, in1=rb[:], op0=ALU.is_ge, op1=ALU.subtract),
                 reads=[RB, RK], writes=[RK])
            yield
            S.op("act", lambda: nc.scalar.activation(out=self.sinb[b][:], in_=kf[:], func=AF.Sin, scale=SINS), reads=[RK], writes=[self.SB[b]])
            yield
            S.op("act", lambda: nc.scalar.activation(out=rb[:], in_=kf[:], func=AF.Abs), reads=[RK], writes=[RB])
            yield
            S.op("act", lambda: nc.scalar.activation(out=self.cosb[b][:], in_=rb[:], func=AF.Sin, scale=-SINS, bias=HALF_PI * (1.0 - 2e-6)),
                 reads=[RB], writes=[self.CB[b]])
            yield
            S.dma("pool", self.tabd[j, 0, :, :], self.cosb[b][:], reads=[self.CB[b]])
            S.dma("pool", self.tabd[j, 1, :, :], self.sinb[b][:], reads=[self.SB[b]])
            yield

    def step(self):
        if self.done:
            return
        try:
            next(self.gen)
        except StopIteration:
            self.done = True

    def drain(self):
        while not self.done:
            self.step()


SEQ = 2048
PARAM_SHAPES = {
    "rel_bias_table": [32, 16], "attn_norm_w": [2, 4096], "w_in": [2, 4096, 10256], "s5_lam_re": [2, 64, 64],
    "s5_lam_im": [2, 64, 64], "s5_log_dt": [2, 64], "s5_b_re": [2, 64, 64, 16], "s5_b_im": [2, 64, 64, 16],
    "s5_c_re": [2, 64, 16, 64], "s5_c_im": [2, 64, 16, 64], "s5_d": [2, 64, 16], "s5_w_glu": [2, 1024, 1024],
    "s5_out_norm_w": [2, 1024], "diff_lam_q1": [2, 64], "diff_lam_k1": [2, 64], "diff_lam_q2": [2, 64],
    "diff_lam_k2": [2, 64], "diff_subln_w": [2, 128], "moba_out_norm_w": [2, 1024], "ssd_conv_w": [2, 4, 1, 2048],
    "ssd_conv_b": [2, 2048], "ssd_dt_bias": [2, 16], "ssd_a_log": [2, 16], "ssd_d": [2, 16], "ssd_norm_w": [2, 1024],
    "w_out": [2, 4096, 4096], "mlp_norm_w": [2, 4096], "w_up": [2, 4096, 16384], "w_down": [2, 16384, 4096],
    "final_norm_w": [4096]}
CONST_SHAPES = {"iota": [2048], "m2c": [128, 2], "m3c": [128, 2], "m4c": [128, 4], "ohm": [2, 32, 16384],
                "negmask": [128, 128], "ident_d": [128, 128], "sel": [8, 1024]}


def build_program(depth=2):
    nc = bass.Bass("TRN2", target_bir_lowering=False)
    dt = lambda n, s, k="ExternalInput", d=F32: nc.dram_tensor(n, s, d, kind=k).ap()
    x = dt("x", [SEQ, D])
    p = {k: dt(k, s) for k, s in PARAM_SHAPES.items()}
    c = {k: dt(k, s) for k, s in CONST_SHAPES.items()}
    out = dt("out", [SEQ, D], "ExternalOutput")
    projF = dt("projF", [INW, SEQ], "Internal"); projT = dt("projT", [SEQ, INW], "Internal")
    mixed = dt("mixed", [SEQ, D], "Internal"); xmid = dt("xmid", [SEQ, D], "Internal")
    xs_ = [dt("xres%d" % i, [SEQ, D], "Internal") for i in range(2)]
    act = dt("act", [16384, 1024], "Internal", BF16)
    btd = dt("btd", [2, 16, 16384], "Internal"); acs = dt("acs", [16, 2048], "Internal"); gtm = dt("gtm", [2048, 1024], "Internal"); gTd = dt("gTd", [1024, 2048], "Internal", BF16); tabd = dt("tabd", [32, 2, 128, 2048], "Internal")
    with ExitStack() as st:
        C = Ctx(nc, st)
        load_consts(C, c["ident_d"])
        build_bias(C, p["rel_bias_table"], c["ohm"], btd)
        for l in range(depth):
            x_in = x if l == 0 else xs_[(l - 1) % 2]
            x_out = xs_[l % 2]
            with ExitStack() as stg_:
                tg = S5TableGen(C, stg_, p["s5_lam_im"][l], p["s5_log_dt"][l], c["iota"], tabd)
                for half in range(2):
                    phase_A(C, x_in[half * 1024:(half + 1) * 1024, :], p["attn_norm_w"][l], p["w_in"][l], 1024, half * 1024, projF, projT,
                            bg=tg.step)
                tg.drain()
                C.S.barrier()
            s5p = {"lam_re": p["s5_lam_re"][l], "lam_im": p["s5_lam_im"][l], "log_dt": p["s5_log_dt"][l], "b_re": p["s5_b_re"][l],
                   "b_im": p["s5_b_im"][l], "c_re": p["s5_c_re"][l], "c_im": p["s5_c_im"][l], "d": p["s5_d"][l],
                   "w_glu": p["s5_w_glu"][l], "onw": p["s5_out_norm_w"][l]}
            s5_mixer(C, projF, s5p, c["iota"], c["m2c"], c["m3c"], c["m4c"], gtm, mixed, gTd, tabd=tabd)
            diff_attn(C, l, projF, projT, btd, c["negmask"],
                      [p["diff_lam_q1"][l], p["diff_lam_k1"][l], p["diff_lam_q2"][l], p["diff_lam_k2"][l]], p["diff_subln_w"][l], mixed)
            moba_attn(C, projF, projT, btd, c["negmask"], c["sel"], p["moba_out_norm_w"][l], mixed)
            ssd_mixer(C, projF, projT, c["negmask"], p["ssd_conv_w"][l], p["ssd_conv_b"][l], p["ssd_dt_bias"][l], p["ssd_a_log"][l],
                      p["ssd_d"][l], p["ssd_norm_w"][l], acs, mixed)
            last = (l == depth - 1)
            for half in range(2):
                phase_C(C, None, x_in, p["w_out"][l], p["mlp_norm_w"][l], p["w_up"][l], p["w_down"][l], 1024, half * 1024,
                        xmid, act, x_out, p["final_norm_w"] if last else None, out if last else None, mixed_tm=mixed)
        C.S.barrier()
    return nc


def kernel(**inputs):
    nc = build_program()
    consts = host_consts()
    params = {k: np.ascontiguousarray(np.asarray(inputs[k], dtype=np.float32)) for k in PARAM_SHAPES}
    x = np.asarray(inputs["x"], dtype=np.float32)
    B = x.shape[0]
    in_maps = []
    for b in range(B):
        m = {"x": np.ascontiguousarray(x[b])}
        m.update(params)
        m.update(consts)
        in_maps.append(m)
    res = run_bass_kernel_spmd(nc, in_maps, core_ids=list(range(B)))
    return np.stack([np.asarray(res.results[b]["out"]) for b in range(B)], axis=0).astype(np.float32)
```

```python
import numpy as np
import concourse.bass as bass
import concourse.mybir as mybir
from concourse.bass_utils import run_bass_kernel_spmd

AF = mybir.ActivationFunctionType
ALU = mybir.AluOpType
AX = mybir.AxisListType
F32 = mybir.dt.float32
F32R = mybir.dt.float32r
BF16 = mybir.dt.bfloat16
I32 = mybir.dt.int32

SAME_ENG_SYNC = True


_UNIQ = [0]


def _sbt(nc, name, shape, dtype):
    _UNIQ[0] += 1
    return nc.sbuf_tensor("%s_u%d" % (name, _UNIQ[0]), shape, dtype)


class Tok:
    __slots__ = ("name", "w", "r")

    def __init__(self, name):
        self.name = name
        self.w = None
        self.r = []


class Sched:
    NDS = 48

    def __init__(self, nc, stack):
        self.nc = nc
        self.engs = {"pe": nc.tensor, "act": nc.scalar, "dve": nc.vector,
                     "pool": nc.gpsimd, "sp": nc.sync}
        self.sem = {}
        for e in ("pe", "act", "dve", "pool"):
            self.sem[e] = stack.enter_context(nc.semaphore("sem_" + e))
        self.cnt = {e: 0 for e in self.sem}
        self.dsem = [stack.enter_context(nc.semaphore("dsem%d" % i)) for i in range(self.NDS)]
        self.dval = [0] * self.NDS
        self.dnext = 0
        self.seen = {e: {} for e in self.engs}
        self.ninstr = 0

    def buf(self, name):
        return Tok(name)

    def bufs(self, name, n):
        return [Tok("%s%d" % (name, i)) for i in range(n)]

    def _wait(self, eng, tok):
        if tok is None:
            return
        kind, key, val = tok
        if kind == "eng" and key == eng and not (SAME_ENG_SYNC and eng in ("act", "dve", "pool")):
            return
        skey = (kind, key)
        if self.seen[eng].get(skey, 0) >= val:
            return
        sem = self.sem[key] if kind == "eng" else self.dsem[key]
        self.engs[eng].wait_ge(sem, val)
        self.seen[eng][skey] = val

    def _deps(self, eng, reads, writes):
        for t in reads:
            self._wait(eng, t.w)
        for t in writes:
            self._wait(eng, t.w)
            for r in t.r:
                self._wait(eng, r)

    def _mark(self, tok, reads, writes):
        for t in reads:
            t.r.append(tok)
            if len(t.r) > 12:
                d = {}
                for r in t.r:
                    k = (r[0], r[1])
                    if k not in d or d[k][2] < r[2]:
                        d[k] = r
                t.r = list(d.values())
        for t in writes:
            t.w = tok
            t.r = []

    def op(self, eng, fn, reads=(), writes=()):
        self._deps(eng, reads, writes)
        ins = fn()
        self.cnt[eng] += 1
        ins.then_inc(self.sem[eng], 1)
        tok = ("eng", eng, self.cnt[eng])
        self._mark(tok, reads, writes)
        self.ninstr += 1
        return tok

    def group(self, eng, fns, reads=(), writes=()):
        self._deps(eng, reads, writes)
        ins = None
        for f in fns:
            ins = f()
            self.ninstr += 1
        self.cnt[eng] += 1
        ins.then_inc(self.sem[eng], 1)
        tok = ("eng", eng, self.cnt[eng])
        self._mark(tok, reads, writes)
        return tok

    def dma(self, q, out, in_, reads=(), writes=(), **kw):
        slot = self.dnext
        self.dnext = (self.dnext + 1) % self.NDS
        if self.dval[slot] > 0:
            self._wait(q, ("dma", slot, self.dval[slot]))
        self._deps(q, reads, writes)
        self.dval[slot] += 16
        self.engs[q].dma_start(out=out, in_=in_, **kw).then_inc(self.dsem[slot], 16)
        tok = ("dma", slot, self.dval[slot])
        self._mark(tok, reads, writes)
        self.ninstr += 1
        return tok

    def wait_tok(self, eng, tok):
        self._wait(eng, tok)

    def barrier(self):
        for e in self.engs:
            for e2 in self.sem:
                if self.cnt[e2] > 0:
                    self._wait(e, ("eng", e2, self.cnt[e2]))
            for s in range(self.NDS):
                if self.dval[s] > 0:
                    self._wait(e, ("dma", s, self.dval[s]))


from contextlib import ExitStack

D = 4096
KC = 32
INW = 10256
EPS = 1e-6


class Ctx:
    def __init__(self, nc, st):
        self.nc = nc
        self.st = st
        self.S = Sched(nc, st)
        self.ps = st.enter_context(nc.psum_tensor("ps", [128, 4096], F32))
        self.PS = self.S.bufs("psbank", 8)
        self.ident = st.enter_context(_sbt(nc, "ident", [128, 128], BF16))
        self.identf = st.enter_context(_sbt(nc, "identf", [128, 128], F32))
        self.IDT = self.S.buf("ident")

    def bank(self, b, n=512):
        return self.ps[:, b * 512:b * 512 + n]

    def bank_bf(self, b):
        return self.ps[:, b * 512:(b + 1) * 512].bitcast(BF16)


def load_consts(C, ident_d):
    nc, S = C.nc, C.S
    S.dma("sp", C.identf[:], ident_d[:, :], writes=[C.IDT])
    S.op("dve", lambda: nc.vector.tensor_copy(out=C.ident[:], in_=C.identf[:]), reads=[C.IDT], writes=[C.IDT])


def build_hT(C, x_rows, nw_d, T, hT, HT, norm=True):
    nc, S = C.nc, C.S
    NT = T // 128
    with ExitStack() as st:
        xs = [st.enter_context(_sbt(nc, "xs%d" % i, [128, D], F32)) for i in range(2)]
        XS = S.bufs("xs", 2)
        junk = st.enter_context(_sbt(nc, "junk", [128, D], BF16))
        JK = S.buf("junk")
        xnb = [st.enter_context(_sbt(nc, "xnb%d" % i, [128, D], BF16)) for i in range(2)]
        XNB = S.bufs("xnb", 2)
        stat = st.enter_context(_sbt(nc, "stat", [128, 4 * NT], F32))
        STT = S.bufs("stat", NT)
        nwT = st.enter_context(_sbt(nc, "nwT", [128, KC], F32))
        NW = S.buf("nwT")
        if norm:
            with nc.allow_non_contiguous_dma(reason="small norm weight transpose"):
                S.dma("sp", nwT[:], nw_d.rearrange("(j p) -> p j", p=128), writes=[NW])
        for tt in range(NT):
            i = tt % 2
            S.dma("sp", xs[i][:], x_rows[tt * 128:(tt + 1) * 128, :], writes=[XS[i]])
            ss = stat[:, 4 * tt:4 * tt + 1]
            sd = stat[:, 4 * tt + 1:4 * tt + 2]
            rs = stat[:, 4 * tt + 2:4 * tt + 3]
            if norm:
                S.op("act", lambda: nc.scalar.activation(out=junk[:], in_=xs[i][:], func=AF.Square, accum_out=ss),
                     reads=[XS[i]], writes=[JK, STT[tt]])
                S.op("act", lambda: nc.scalar.activation(out=sd, in_=ss, func=AF.Sqrt, scale=1.0 / D, bias=EPS),
                     reads=[STT[tt]], writes=[STT[tt]])
                S.op("dve", lambda: nc.vector.reciprocal(out=rs, in_=sd), reads=[STT[tt]], writes=[STT[tt]])
                S.op("act", lambda: nc.scalar.activation(out=xnb[i][:], in_=xs[i][:], func=AF.Copy, scale=rs),
                     reads=[XS[i], STT[tt]], writes=[XNB[i]])
            else:
                S.op("act", lambda: nc.scalar.copy(out=xnb[i][:], in_=xs[i][:]), reads=[XS[i]], writes=[XNB[i]])
            for g in range(4):
                b = (tt % 2) * 4 + g
                pb = C.bank_bf(b)
                fns = []
                for jj in range(8):
                    j = g * 8 + jj
                    fns.append(lambda j=j, jj=jj, pb=pb: nc.tensor.transpose(
                        pb[:, jj * 128:(jj + 1) * 128], xnb[i][:, j * 128:(j + 1) * 128], C.ident[:]))
                S.group("pe", fns, reads=[XNB[i], C.IDT], writes=[C.PS[b]])
                if norm:
                    S.op("dve", lambda g=g, pb=pb: nc.vector.tensor_tensor(
                        out=hT[:, g * 8:(g + 1) * 8, tt * 128:(tt + 1) * 128],
                        in0=pb.rearrange("p (j t) -> p j t", j=8),
                        in1=nwT[:, g * 8:(g + 1) * 8].unsqueeze(2).to_broadcast([128, 8, 128]),
                        op=ALU.mult), reads=[C.PS[b], NW], writes=[HT])
                else:
                    S.op("dve", lambda g=g, pb=pb: nc.vector.tensor_copy(
                        out=hT[:, g * 8:(g + 1) * 8, tt * 128:(tt + 1) * 128],
                        in_=pb.rearrange("p (j t) -> p j t", j=8)), reads=[C.PS[b]], writes=[HT])
        S.barrier()


def dense(C, AT, ATK, kc_n, T, w_d, blocks, evac, name="d", pre=None, a_d=None, bg=None):
    nc, S = C.nc, C.S
    KG = 4
    ngr = kc_n // KG
    NH = T // 512
    NT = T // 128
    with ExitStack() as st:
        NB = 5 if a_d is None else 4
        ws = [st.enter_context(_sbt(nc, "%s_ws%d" % (name, i), [128, KG, 512], F32)) for i in range(NB)]
        wb = [st.enter_context(_sbt(nc, "%s_wb%d" % (name, i), [128, KG, 512], BF16)) for i in range(NB)]
        WS = S.bufs(name + "ws", NB)
        WB = S.bufs(name + "wb", NB)
        if a_d is not None:
            ab = [st.enter_context(_sbt(nc, "%s_ab%d" % (name, i), [128, KG, T], BF16)) for i in range(NB)]
            AB = S.bufs(name + "ab", NB)
            av = a_d.rearrange("(c p) t -> p c t", p=128)
        it = 0
        wv = w_d.rearrange("(c p) n -> p c n", p=128)
        for (c0, width, mode) in blocks:
            if mode == "fm":
                mts = [(m0, min(128, width - m0)) for m0 in range(0, width, 128)]
                assert len(mts) * NH <= 8
            else:
                assert NT <= 8
            if pre is not None:
                pre(c0)
            for g in range(ngr):
                i = it % NB
                it += 1
                if bg is not None:
                    bg()
                S.dma("sp", ws[i][:, :, 0:width], wv[:, g * KG:(g + 1) * KG, c0:c0 + width], writes=[WS[i]])
                if a_d is not None:
                    S.dma("sp", ab[i][:], av[:, g * KG:(g + 1) * KG, :], writes=[AB[i]])
                if it % 2 == 0:
                    S.op("act", lambda i=i: nc.scalar.copy(out=wb[i][:, :, 0:width], in_=ws[i][:, :, 0:width]),
                         reads=[WS[i]], writes=[WB[i]])
                else:
                    S.op("dve", lambda i=i: nc.vector.tensor_copy(out=wb[i][:, :, 0:width], in_=ws[i][:, :, 0:width]),
                         reads=[WS[i]], writes=[WB[i]])
                per_bank = {}
                for j in range(KG):
                    kc = g * KG + j
                    first = (kc == 0)
                    last = (kc == kc_n - 1)
                    if mode == "fm":
                        for mi, (m0, mw) in enumerate(mts):
                            for h in range(NH):
                                bnk = mi * NH + h
                                per_bank.setdefault(bnk, []).append(
                                    lambda i=i, j=j, m0=m0, mw=mw, h=h, bnk=bnk, kc=kc, first=first, last=last:
                                    nc.tensor.matmul(C.bank(bnk)[0:mw, :], wb[i][:, j, m0:m0 + mw],
                                                     AT[:, kc, h * 512:(h + 1) * 512], start=first, stop=last))
                    else:
                        for tt in range(NT):
                            if a_d is None:
                                per_bank.setdefault(tt, []).append(
                                    lambda i=i, j=j, tt=tt, kc=kc, first=first, last=last:
                                    nc.tensor.matmul(C.bank(tt)[:, 0:width], AT[:, kc, tt * 128:(tt + 1) * 128],
                                                     wb[i][:, j, 0:width], start=first, stop=last))
                            else:
                                per_bank.setdefault(tt, []).append(
                                    lambda i=i, j=j, tt=tt, kc=kc, first=first, last=last:
                                    nc.tensor.matmul(C.bank(tt)[:, 0:width], ab[i][:, j, tt * 128:(tt + 1) * 128],
                                                     wb[i][:, j, 0:width], start=first, stop=last))
                rd = [WB[i]] + ([ATK] if a_d is None else [AB[i]])
                if g == 0:
                    for bnk in sorted(per_bank):
                        S.group("pe", per_bank[bnk], reads=rd, writes=[C.PS[bnk]])
                else:
                    fns = []
                    nb_ = sorted(per_bank)
                    for j in range(KG):
                        for bnk in nb_:
                            fns.append(per_bank[bnk][j])
                    S.group("pe", fns, reads=rd, writes=[C.PS[bnk] for bnk in nb_])
            if mode == "fm":
                for mi, (m0, mw) in enumerate(mts):
                    for h in range(NH):
                        bnk = mi * NH + h
                        evac(mode, c0, (m0, mw, h), C.bank(bnk)[0:mw, :], C.PS[bnk])
            else:
                for tt in range(NT):
                    evac(mode, c0, tt, C.bank(tt)[:, 0:width], C.PS[tt])
        S.barrier()


class Stager:
    def __init__(self, C, st, name, n=4, dtype=F32, width=512):
        self.C = C
        self.t = [st.enter_context(_sbt(C.nc, "%s%d" % (name, i), [128, width], dtype)) for i in range(n)]
        self.T = C.S.bufs(name, n)
        self.i = 0
        self.n = n

    def next(self):
        i = self.i % self.n
        self.i += 1
        return self.t[i], self.T[i], ("dve" if self.i % 2 == 0 else "act")


def copy_op(C, eng, out, in_, reads, writes):
    nc, S = C.nc, C.S
    if eng == "act":
        return S.op("act", lambda: nc.scalar.copy(out=out, in_=in_), reads=reads, writes=writes)
    elif eng == "dve":
        return S.op("dve", lambda: nc.vector.tensor_copy(out=out, in_=in_), reads=reads, writes=writes)
    else:
        return S.op("pool", lambda: nc.gpsimd.tensor_copy(out=out, in_=in_), reads=reads, writes=writes)


def win_blocks():
    bl = []
    fm_ranges = [(0, 3072), (4096, 6144), (8192, 10240)]
    tm_ranges = [(3072, 4096), (6144, 8192)]
    for a, b in fm_ranges:
        for c in range(a, b, 512):
            bl.append((c, 512, "fm"))
    for a, b in tm_ranges:
        for c in range(a, b, 512):
            bl.append((c, 512, "tm"))
    bl.append((10240, 16, "fm"))
    return bl


def phase_A(C, x_rows, nw_d, w_d, T, tok0, projF, projT, blocks=None, bg=None):
    nc, S = C.nc, C.S
    with ExitStack() as st:
        hT = st.enter_context(_sbt(nc, "hT", [128, KC, T], BF16))
        HT = S.buf("hT")
        build_hT(C, x_rows, nw_d, T, hT, HT)
        stg = Stager(C, st, "stgA", 8)

        def evac(mode, c0, idx, pap, ptok):
            t, tk, eng = stg.next()
            if mode == "fm":
                m0, mw, h = idx
                copy_op(C, eng, t[0:mw, :], pap, [ptok], [tk])
                S.dma("pool", projF[c0 + m0:c0 + m0 + mw, tok0 + h * 512:tok0 + (h + 1) * 512], t[0:mw, :], reads=[tk])
            else:
                tt = idx
                w = pap.shape[1]
                copy_op(C, eng, t[:, 0:w], pap, [ptok], [tk])
                S.dma("pool", projT[tok0 + tt * 128:tok0 + (tt + 1) * 128, c0:c0 + w], t[:, 0:w], reads=[tk])

        dense(C, hT, HT, KC, T, w_d, blocks or win_blocks(), evac, name="A", bg=bg)


def load_AT(C, src_d, kc_n, T, t0, AT, ATK, name="ld"):
    nc, S = C.nc, C.S
    sv = src_d.rearrange("(c p) t -> p c t", p=128)
    with ExitStack() as st:
        tmp = [st.enter_context(_sbt(nc, "%s_t%d" % (name, i), [128, 4, T], F32)) for i in range(2)]
        TM = S.bufs(name + "t", 2)
        for g in range(kc_n // 4):
            i = g % 2
            S.dma("sp", tmp[i][:], sv[:, g * 4:(g + 1) * 4, t0:t0 + T], writes=[TM[i]])
            copy_op(C, ["act", "pool", "dve"][g % 3], AT[:, g * 4:(g + 1) * 4, :], tmp[i][:], [TM[i]], [ATK])
        S.barrier()


def phase_C(C, mixT_d, x_d, wout_d, nw2_d, wup_d, wdown_d, T, tok0, xmid_d, act_d, xout_d,
            final_nw_d=None, out_d=None, mixed_tm=None):
    nc, S = C.nc, C.S
    FF = 16384
    with ExitStack() as st:
        mT = st.enter_context(_sbt(nc, "mT", [128, KC, T], BF16))
        MT = S.buf("mT")
        if mixed_tm is not None:
            build_hT(C, mixed_tm[tok0:tok0 + T, :], None, T, mT, MT, norm=False)
        else:
            load_AT(C, mixT_d, KC, T, tok0, mT, MT, name="ldm")
        stg = Stager(C, st, "stgC", 8)
        xin = Stager(C, st, "xinC", 8)
        pend = {}

        def pre1(c0):
            for tt in range(T // 128):
                xi, xk, _ = xin.next()
                rows = slice(tok0 + tt * 128, tok0 + (tt + 1) * 128)
                S.dma("pool", xi[:], x_d[rows, c0:c0 + 512], writes=[xk])
                pend[tt] = (xi, xk)

        def evac1(mode, c0, tt, pap, ptok):
            t, tk, _ = stg.next()
            xi, xk = pend[tt]
            rows = slice(tok0 + tt * 128, tok0 + (tt + 1) * 128)
            if tt % 2 == 0:
                S.op("dve", lambda: nc.vector.tensor_tensor(out=t[:], in0=pap, in1=xi[:], op=ALU.add),
                     reads=[ptok, xk], writes=[tk])
            else:
                S.op("act", lambda: nc.scalar.copy(out=t[:], in_=pap), reads=[ptok], writes=[tk])
                S.op("pool", lambda: nc.gpsimd.tensor_tensor(out=t[:], in0=t[:], in1=xi[:], op=ALU.add), reads=[tk, xk], writes=[tk])
            S.dma("pool", xmid_d[rows, c0:c0 + 512], t[:], reads=[tk])

        dense(C, mT, MT, KC, T, wout_d, [(c, 512, "tm") for c in range(0, D, 512)], evac1, name="O", pre=pre1)
    with ExitStack() as st:
        h2T = st.enter_context(_sbt(nc, "h2T", [128, KC, T], BF16))
        H2 = S.buf("h2T")
        build_hT(C, xmid_d[tok0:tok0 + T, :], nw2_d, T, h2T, H2)
        stg = Stager(C, st, "stgU", 8)
        stb = Stager(C, st, "stbU", 8, dtype=BF16)

        def evac2(mode, c0, idx, pap, ptok):
            m0, mw, h = idx
            t, tk, _ = stg.next()
            tb, tbk, _ = stb.next()
            if stg.i % 2 == 0:
                S.op("act", lambda: nc.scalar.activation(out=t[:], in_=pap, func=AF.Relu), reads=[ptok], writes=[tk])
            else:
                S.op("dve", lambda: nc.vector.tensor_scalar(out=t[:], in0=pap, scalar1=0.0, scalar2=None, op0=ALU.max), reads=[ptok], writes=[tk])
            S.op("pool", lambda: nc.gpsimd.tensor_tensor(out=tb[:], in0=t[:], in1=t[:], op=ALU.mult), reads=[tk], writes=[tbk])
            S.dma("pool", act_d[c0 + m0:c0 + m0 + mw, h * 512:(h + 1) * 512], tb[:], reads=[tbk])

        dense(C, h2T, H2, KC, T, wup_d, [(c, 512, "fm") for c in range(0, FF, 512)], evac2, name="U")
    with ExitStack() as st:
        stg = Stager(C, st, "stgD", 8)
        xin = Stager(C, st, "xinD", 8)
        pend3 = {}

        def pre3(c0):
            for tt in range(T // 128):
                xi, xk, _ = xin.next()
                rows = slice(tok0 + tt * 128, tok0 + (tt + 1) * 128)
                S.dma("pool", xi[:], xmid_d[rows, c0:c0 + 512], writes=[xk])
                pend3[tt] = (xi, xk)

        def evac3(mode, c0, tt, pap, ptok):
            t, tk, _ = stg.next()
            xi, xk = pend3[tt]
            rows = slice(tok0 + tt * 128, tok0 + (tt + 1) * 128)
            if tt % 2 == 0:
                S.op("dve", lambda: nc.vector.tensor_tensor(out=t[:], in0=pap, in1=xi[:], op=ALU.add),
                     reads=[ptok, xk], writes=[tk])
            else:
                S.op("act", lambda: nc.scalar.copy(out=t[:], in_=pap), reads=[ptok], writes=[tk])
                S.op("pool", lambda: nc.gpsimd.tensor_tensor(out=t[:], in0=t[:], in1=xi[:], op=ALU.add), reads=[tk, xk], writes=[tk])
            S.dma("pool", xout_d[rows, c0:c0 + 512], t[:], reads=[tk])

        dense(C, None, None, FF // 128, T, wdown_d, [(c, 512, "tm") for c in range(0, D, 512)], evac3, name="Dn", pre=pre3, a_d=act_d)
    if final_nw_d is not None:
        final_norm(C, xout_d, final_nw_d, T, tok0, out_d)


def final_norm(C, xin_d, nw_d, T, tok0, out_d):
    nc, S = C.nc, C.S
    NT = T // 128
    with ExitStack() as st:
        xs = [st.enter_context(_sbt(nc, "fxs%d" % i, [128, D], F32)) for i in range(2)]
        XS = S.bufs("fxs", 2)
        ys = [st.enter_context(_sbt(nc, "fys%d" % i, [128, D], F32)) for i in range(2)]
        YS = S.bufs("fys", 2)
        nwb = st.enter_context(_sbt(nc, "fnw", [128, D], F32))
        NW = S.buf("fnw")
        stat = st.enter_context(_sbt(nc, "fstat", [128, 4 * NT], F32))
        STT = S.bufs("fstat", NT)
        S.dma("sp", nwb[:], nw_d.partition_broadcast(128), writes=[NW])
        for tt in range(NT):
            i = tt % 2
            rows = slice(tok0 + tt * 128, tok0 + (tt + 1) * 128)
            S.dma("sp", xs[i][:], xin_d[rows, :], writes=[XS[i]])
            ss = stat[:, 4 * tt:4 * tt + 1]
            sd = stat[:, 4 * tt + 1:4 * tt + 2]
            rs = stat[:, 4 * tt + 2:4 * tt + 3]
            S.op("act", lambda: nc.scalar.activation(out=ys[i][:], in_=xs[i][:], func=AF.Square, accum_out=ss),
                 reads=[XS[i]], writes=[YS[i], STT[tt]])
            S.op("act", lambda: nc.scalar.activation(out=sd, in_=ss, func=AF.Sqrt, scale=1.0 / D, bias=EPS),
                 reads=[STT[tt]], writes=[STT[tt]])
            S.op("dve", lambda: nc.vector.reciprocal(out=rs, in_=sd), reads=[STT[tt]], writes=[STT[tt]])
            S.op("dve", lambda: nc.vector.scalar_tensor_tensor(out=ys[i][:], in0=xs[i][:], scalar=rs, in1=nwb[:],
                                                               op0=ALU.mult, op1=ALU.mult),
                 reads=[XS[i], STT[tt], NW], writes=[YS[i]])
            S.dma("sp", out_d[rows, :], ys[i][:], reads=[YS[i]])
        S.barrier()


def host_consts():
    import math
    def bucket(n):
        n = max(n, 0)
        if n < 16:
            return n
        lr = np.log(np.float32(max(n, 1)) / np.float32(16)) / np.float32(math.log(128 / 16))
        return min(16 + int(np.float32(lr) * np.float32(16)), 31)
    bk = np.array([bucket(n) for n in range(0, 256)])
    ohm = np.zeros((2, 32, 128, 128), np.float32)
    kl = np.arange(128)[:, None]
    ql = np.arange(128)[None, :]
    for t in range(2):
        dist = t * 128 + ql - kl
        valid = dist >= 0
        b = bk[np.clip(dist, 0, 255)]
        for bb in range(32):
            ohm[t, bb] = ((b == bb) & valid).astype(np.float32)
        ohm[t, 31] -= valid.astype(np.float32)
    negmask = np.where(ql < kl, -30000.0, 0.0).astype(np.float32)
    sel = np.zeros((8, 8, 128), np.float32)
    for n in range(8):
        sel[n, n, :] = 1.0
    q = np.arange(128)
    m2c = np.stack([(q // 64 == 0), (q // 64 == 1)], 1).astype(np.float32)
    m3c = np.stack([((q // 16) % 2 == 0), ((q // 16) % 2 == 1)], 1).astype(np.float32)
    m4c = np.stack([(q // 32 == k) for k in range(4)], 1).astype(np.float32)
    return {"iota": np.arange(2048, dtype=np.float32), "m2c": m2c, "m3c": m3c, "m4c": m4c, "ohm": ohm.reshape(2, 32, 16384), "negmask": negmask, "ident_d": np.eye(128, dtype=np.float32),
            "sel": sel.reshape(8, 1024)}


def build_bias(C, tbl_d, ohm_d, btd):
    nc, S = C.nc, C.S
    with ExitStack() as st:
        tb = st.enter_context(_sbt(nc, "bb_tbl", [32, 16], F32))
        TB = S.buf("bb_tbl")
        oh = [st.enter_context(_sbt(nc, "bb_oh%d" % i, [32, 16384], F32)) for i in range(2)]
        OH = S.bufs("bb_oh", 2)
        stg = Stager(C, st, "bb_stg", 4)
        S.dma("sp", tb[:], tbl_d[:, :], writes=[TB])
        for t in range(2):
            S.dma("sp", oh[t][:], ohm_d[t, :, :], writes=[OH[t]])
        k = 0
        for t in range(2):
            for c in range(32):
                b = k % 8
                k += 1
                S.op("pe", lambda: nc.tensor.matmul(C.bank(b)[0:16, :], tb[:], oh[t][:, c * 512:(c + 1) * 512],
                                                    start=True, stop=True), reads=[TB, OH[t]], writes=[C.PS[b]])
                sg, sk, eng = stg.next()
                copy_op(C, eng, sg[0:16, :], C.bank(b)[0:16, :], [C.PS[b]], [sk])
                S.dma("sp", btd[t, :, c * 512:(c + 1) * 512], sg[0:16, :], reads=[sk])
        S.barrier()


def load_bias_tiles(C, btd, negmask_sb, NM, head, inv_scale, T0b, T1b, TB_tok, tmp, TMP):
    nc, S = C.nc, C.S
    for t, dst in ((0, T0b), (1, T1b)):
        S.dma("sp", tmp[:], btd[t, head, :].rearrange("(k q) -> k q", q=128), writes=[TMP])
        if t == 0:
            S.op("dve", lambda: nc.vector.tensor_tensor(out=tmp[:], in0=tmp[:], in1=negmask_sb[:], op=ALU.add),
                 reads=[TMP, NM], writes=[TMP])
        S.op("act", lambda: nc.scalar.mul(out=dst[:], in_=tmp[:], mul=inv_scale), reads=[TMP], writes=[TB_tok])


def attention(C, ptl, name, qT, kT, QK, vb, VB, dv, T0b, T1b, TBK, transform, on_O, extra=None, nqb=4):
    nc, S = C.nc, C.S
    pt, PT = ptl
    assert len(pt) >= 4
    SBANKS = (0, 1, 6)
    vw = vb.shape[2]
    iters = [(qb, kt) for qb in range(nqb) for kt in range(4 * qb + 4)]

    def emit_S(n):
        qb, kt = iters[n]
        qs0 = max(0, kt - 4 * qb)
        c0 = qs0 * 128
        sb = SBANKS[n % 3]
        bank = C.bank(sb)
        ops = [(bank[:, c0:512], kT[:, kt * 128:(kt + 1) * 128], qT[:, qb * 512 + c0:qb * 512 + 512])]
        rd = [QK]
        for qs in range(qs0, 4):
            d = 4 * qb + qs - kt
            if d == 0 and T0b is not None:
                ops.append((bank[:, qs * 128:(qs + 1) * 128], C.ident[:], T0b[:]))
                rd += [TBK, C.IDT]
            if d == 1 and T1b is not None:
                ops.append((bank[:, qs * 128:(qs + 1) * 128], C.ident[:], T1b[:]))
                rd += [TBK, C.IDT]
        if extra is not None:
            eo, er = extra(kt, qb, qs0, bank)
            ops += eo
            rd += er
        fns = []
        for oi, (o, l, r) in enumerate(ops):
            fns.append(lambda o=o, l=l, r=r, oi=oi: nc.tensor.matmul(o, l, r, start=(oi == 0), stop=(oi == len(ops) - 1)))
        S.group("pe", fns, reads=rd, writes=[C.PS[sb]])

    emit_S(0)
    if len(iters) > 1:
        emit_S(1)
    for n, (qb, kt) in enumerate(iters):
        if n + 2 < len(iters):
            emit_S(n + 2)
        qs0 = max(0, kt - 4 * qb)
        c0 = qs0 * 128
        sb = SBANKS[n % 3]
        i = n % 4
        bank = C.bank(sb)
        transform(kt, qb, c0, bank[:, c0:512], pt[i][:, c0:512], [C.PS[sb]], [PT[i]])
        fns = []
        wr = []
        for qs in range(qs0, 4):
            qt = 4 * qb + qs
            fns.append(lambda qs=qs, qt=qt: nc.tensor.matmul(C.bank(2 + qs)[:, 0:vw], pt[i][:, qs * 128:(qs + 1) * 128],
                                                            vb[:, kt, :], start=(kt == 0), stop=(kt == qt)))
            wr.append(C.PS[2 + qs])
        S.group("pe", fns, reads=[PT[i], VB], writes=wr)
        if kt >= 4 * qb:
            on_O(kt, C.bank(2 + (kt - 4 * qb))[:, 0:vw], C.PS[2 + (kt - 4 * qb)])


def exp_transform(C, scale):
    nc, S = C.nc, C.S

    def tr(kt, qb, c0, pap, oap, reads, writes):
        S.op("act", lambda: nc.scalar.activation(out=oap, in_=pap, func=AF.Exp, scale=scale), reads=reads, writes=writes)
    return tr


def rstd_ops(C, ss, sd, rs, n, TK):
    nc, S = C.nc, C.S
    S.op("dve", lambda: nc.vector.tensor_scalar(out=sd, in0=ss, scalar1=1.0 / n, scalar2=EPS, op0=ALU.mult, op1=ALU.add), reads=[TK], writes=[TK])
    S.op("act", lambda: nc.scalar.activation(out=sd, in_=sd, func=AF.Ln), reads=[TK], writes=[TK])
    S.op("act", lambda: nc.scalar.activation(out=rs, in_=sd, func=AF.Exp, scale=-0.5), reads=[TK], writes=[TK])


def diff_attn(C, layer, projF, projT, btd, negmask_d, lam_d, subw_d, mixed_d):
    import math
    nc, S = C.nc, C.S
    lam_init = 0.8 - 0.6 * math.exp(-0.3 * layer)
    scale = 64 ** -0.5
    with ExitStack() as st:
        sb = lambda n, s, d=F32: st.enter_context(_sbt(nc, "df_" + n, s, d))
        negm = sb("negm", [128, 128]); NM = S.buf("negm")
        S.dma("sp", negm[:], negmask_d[:, :], writes=[NM])
        lv = sb("lv", [128, 4, 64]); LV = S.buf("lv")
        for i in range(4):
            S.dma("sp", lv[:, i, :], lam_d[i].partition_broadcast(128), writes=[LV])
        ls = sb("ls", [128, 8]); LS = S.buf("ls")
        junk = sb("junk", [128, 64])
        for m in range(2):
            S.op("dve", lambda: nc.vector.tensor_tensor(out=junk[:], in0=lv[:, 2 * m, :], in1=lv[:, 2 * m + 1, :], op=ALU.mult),
                 reads=[LV], writes=[LS])
            S.op("dve", lambda: nc.vector.tensor_reduce(out=ls[:, m:m + 1], in_=junk[:], axis=AX.X, op=ALU.add), reads=[LS], writes=[LS])
            S.op("act", lambda: nc.scalar.activation(out=ls[:, 2 + m:3 + m], in_=ls[:, m:m + 1], func=AF.Exp), reads=[LS], writes=[LS])
        S.op("dve", lambda: nc.vector.tensor_tensor(out=ls[:, 4:5], in0=ls[:, 3:4], in1=ls[:, 2:3], op=ALU.subtract), reads=[LS], writes=[LS])
        S.op("dve", lambda: nc.vector.tensor_scalar(out=ls[:, 5:6], in0=ls[:, 4:5], scalar1=-lam_init, scalar2=None, op0=ALU.add),
             reads=[LS], writes=[LS])
        nlam = ls[:, 5:6]
        subw = sb("subw", [128, 128]); SW = S.buf("subw")
        S.dma("sp", subw[:], subw_d.partition_broadcast(128), writes=[SW])
        S.op("act", lambda: nc.scalar.mul(out=subw[:], in_=subw[:], mul=(1.0 - lam_init)), reads=[SW], writes=[SW])
        qf = sb("qf", [128, 2048]); QF = S.buf("qf")
        kf = sb("kf", [128, 2048]); KF = S.buf("kf")
        vf = sb("vf", [128, 16, 128]); VF = S.buf("vf")
        qT = sb("qT", [128, 2048], BF16)
        kTm = [sb("kT%d" % m, [128, 2048], BF16) for m in range(2)]
        QK = S.buf("qk")
        vb = sb("vb", [128, 16, 129], BF16); VB = S.buf("vb")
        T0b = sb("T0b", [128, 128], BF16); T1b = sb("T1b", [128, 128], BF16); TBK = S.buf("tb")
        tmp = sb("tmp", [128, 128]); TMP = S.buf("tmp")
        o1 = sb("o1", [128, 16, 128]); O1 = S.buf("o1")
        yb = [sb("y%d" % i, [128, 128]) for i in range(2)]; YB = S.bufs("dfy", 2)
        stt = sb("stt", [128, 8 * 32]); STT = S.buf("stt")
        ssq = sb("ssq", [128, 48]); SSQ = S.buf("df_ssq")
        S.op("pool", lambda: nc.gpsimd.memset(vb[:, :, 128:129], 1.0), writes=[VB])
        ptl = ([sb("pt%d" % i, [128, 512], BF16) for i in range(4)], S.bufs("df_pt", 4))
        for m in range(2):
            S.op("pool", lambda: nc.gpsimd.memset(kTm[m][:], 0.0), writes=[QK])
        for h in range(8):
            S.dma("sp", qf[:], projF[1024 + h * 128:1024 + (h + 1) * 128, :], writes=[QF])
            S.dma("sp", kf[:], projF[2048 + h * 128:2048 + (h + 1) * 128, :], writes=[KF])
            S.dma("sp", vf[:], projT[:, 3072 + h * 128:3072 + (h + 1) * 128].rearrange("(t p) d -> p t d", p=128), writes=[VF])
            S.op("act", lambda: nc.scalar.copy(out=qT[:], in_=qf[:]), reads=[QF], writes=[QK])
            S.op("dve", lambda: nc.vector.tensor_copy(out=kTm[0][0:64, :], in_=kf[0:64, :]), reads=[KF], writes=[QK])
            S.op("dve", lambda: nc.vector.tensor_copy(out=kTm[1][64:128, :], in_=kf[64:128, :]), reads=[KF], writes=[QK])
            S.op("pool", lambda: nc.gpsimd.tensor_copy(out=vb[:, :, 0:128], in_=vf[:]), reads=[VF], writes=[VB])
            load_bias_tiles(C, btd, negm, NM, h, 1.0 / scale, T0b, T1b, TBK, tmp, TMP)
            for m in range(2):
                def on_O(qt, oap, otok, m=m, h=h):
                    c = (qt % 32) * 8
                    rc = stt[:, c:c + 1]
                    S.op("dve", lambda: nc.vector.reciprocal(out=rc, in_=oap[:, 128:129]), reads=[otok], writes=[STT])
                    if m == 0:
                        S.op("dve", lambda: nc.vector.tensor_scalar(out=o1[:, qt, :], in0=oap[:, 0:128], scalar1=rc, scalar2=None, op0=ALU.mult),
                             reads=[otok, STT], writes=[O1])
                    else:
                        r2 = stt[:, c + 1:c + 2]
                        S.op("dve", lambda: nc.vector.tensor_tensor(out=r2, in0=rc, in1=nlam, op=ALU.mult), reads=[STT, LS], writes=[STT])
                        S.op("dve", lambda: nc.vector.scalar_tensor_tensor(out=o1[:, qt, :], in0=oap[:, 0:128], scalar=r2, in1=o1[:, qt, :],
                                                                           op0=ALU.mult, op1=ALU.add), reads=[otok, STT, O1], writes=[O1])
                        S.op("dve", lambda: nc.vector.tensor_tensor(out=tmp[:], in0=o1[:, qt, :], in1=o1[:, qt, :], op=ALU.mult), reads=[O1], writes=[TMP])
                        S.op("dve", lambda: nc.vector.tensor_reduce(out=ssq[:, qt:qt + 1], in_=tmp[:], axis=AX.X, op=ALU.add), reads=[TMP], writes=[SSQ])
                attention(C, ptl, "df%d_%d" % (h, m), qT, kTm[m], QK, vb, VB, 128, T0b, T1b, TBK,
                          exp_transform(C, scale), on_O)
            S.op("dve", lambda: nc.vector.tensor_scalar(out=ssq[:, 16:32], in0=ssq[:, 0:16], scalar1=1.0 / 128, scalar2=EPS, op0=ALU.mult, op1=ALU.add),
                 reads=[SSQ], writes=[SSQ])
            S.op("act", lambda: nc.scalar.activation(out=ssq[:, 16:32], in_=ssq[:, 16:32], func=AF.Ln), reads=[SSQ], writes=[SSQ])
            S.op("act", lambda: nc.scalar.activation(out=ssq[:, 32:48], in_=ssq[:, 16:32], func=AF.Exp, scale=-0.5), reads=[SSQ], writes=[SSQ])
            for qt in range(16):
                y = yb[qt % 2]; Y = YB[qt % 2]
                S.op("dve", lambda: nc.vector.scalar_tensor_tensor(out=y[:], in0=o1[:, qt, :], scalar=ssq[:, 32 + qt:33 + qt], in1=subw[:],
                                                                   op0=ALU.mult, op1=ALU.mult), reads=[O1, SSQ, SW], writes=[Y])
                S.dma("pool", mixed_d[qt * 128:(qt + 1) * 128, 1024 + h * 128:1024 + (h + 1) * 128], y[:], reads=[Y])
        S.barrier()


def moba_attn(C, projF, projT, btd, negmask_d, sel_d, monw_d, mixed_d):
    nc, S = C.nc, C.S
    scale = 128 ** -0.5
    with ExitStack() as st:
        sb = lambda n, s, d=F32: st.enter_context(_sbt(nc, "mb_" + n, s, d))
        negm = sb("negm", [128, 128]); NM = S.buf("mb_negm")
        S.dma("sp", negm[:], negmask_d[:, :], writes=[NM])
        self = sb("self", [8, 1024]); selb = sb("selb", [128, 8, 128], BF16); SEL = S.buf("mb_sel")
        S.dma("sp", self[:], sel_d[:, :], writes=[SEL])
        S.op("pool", lambda: nc.gpsimd.memset(selb[:], 0.0), writes=[SEL])
        S.op("dve", lambda: nc.vector.tensor_copy(out=selb[0:8, :, :], in_=self[:].rearrange("p (n k) -> p n k", k=128)),
             reads=[SEL], writes=[SEL])
        monw = sb("monw", [128, 1024]); MW = S.buf("mb_monw")
        S.dma("sp", monw[:], monw_d.partition_broadcast(128), writes=[MW])
        qf = sb("qf", [128, 2048]); QF = S.buf("mb_qf")
        kf = sb("kf", [128, 2048]); KF = S.buf("mb_kf")
        vf = sb("vf", [128, 16, 128]); VF = S.buf("mb_vf")
        qT = sb("qT", [128, 2048], BF16); kT = sb("kT", [128, 2048], BF16); QK = S.buf("mb_qk")
        vb = sb("vb", [128, 16, 129], BF16); VB = S.buf("mb_vb")
        T0b = sb("T0b", [128, 128], BF16); T1b = sb("T1b", [128, 128], BF16); TBK = S.buf("mb_tb")
        tmp = sb("tmp", [128, 128]); TMP = S.buf("mb_tmp")
        km = sb("km", [128, 8]); KM = S.buf("mb_km")
        g8 = sb("g8", [128, 8]); mx = sb("mx", [128, 8]); mq = sb("mq", [128, 8]); G8 = S.buf("mb_g8")
        MTb = sb("MTb", [128, 2048], BF16); MT = S.buf("mb_MT")
        mo = sb("mo", [128, 16, 1024]); MO = S.buf("mb_mo")
        stt = sb("stt", [128, 64]); STT = S.buf("mb_stt")
        ptl = ([sb("pt%d" % i, [128, 512], BF16) for i in range(4)], S.bufs("mb_pt", 4))
        S.op("pool", lambda: nc.gpsimd.memset(vb[:, :, 128:129], 1.0), writes=[VB])
        S.op("pool", lambda: nc.gpsimd.memset(MTb[:], 0.0), writes=[MT])
        for h in range(8):
            S.dma("sp", qf[:], projF[4096 + h * 128:4096 + (h + 1) * 128, :], writes=[QF])
            S.dma("sp", kf[:], projF[5120 + h * 128:5120 + (h + 1) * 128, :], writes=[KF])
            S.dma("sp", vf[:], projT[:, 6144 + h * 128:6144 + (h + 1) * 128].rearrange("(t p) d -> p t d", p=128), writes=[VF])
            S.op("act", lambda: nc.scalar.copy(out=qT[:], in_=qf[:]), reads=[QF], writes=[QK])
            S.op("dve", lambda: nc.vector.tensor_copy(out=kT[:], in_=kf[:]), reads=[KF], writes=[QK])
            S.op("pool", lambda: nc.gpsimd.tensor_copy(out=vb[:, :, 0:128], in_=vf[:]), reads=[VF], writes=[VB])
            load_bias_tiles(C, btd, negm, NM, 8 + h, 1.0 / scale, T0b, T1b, TBK, tmp, TMP)
            S.op("dve", lambda: nc.vector.tensor_reduce(out=km[:], in_=kf[:].rearrange("p (n k) -> p n k", k=256), axis=AX.X, op=ALU.add),
                 reads=[KF], writes=[KM])
            for qt in range(8, 16):
                own = qt // 2
                S.op("pe", lambda: nc.tensor.matmul(C.bank(6)[:, 0:8], qf[:, qt * 128:(qt + 1) * 128], km[:], start=True, stop=True),
                     reads=[QF, KM], writes=[C.PS[6]])
                S.op("dve", lambda: nc.vector.tensor_copy(out=g8[:], in_=C.bank(6)[:, 0:8]), reads=[C.PS[6]], writes=[G8])
                S.op("dve", lambda: nc.vector.memset(g8[:, own:8], -1e30), reads=[], writes=[G8])
                S.op("dve", lambda: nc.vector.max(out=mx[:], in_=g8[:]), reads=[G8], writes=[G8])
                S.op("dve", lambda: nc.vector.tensor_scalar(out=mq[:], in0=g8[:], scalar1=mx[:, 2:3], scalar2=-30000.0,
                                                            op0=ALU.is_lt, op1=ALU.mult), reads=[G8], writes=[G8])
                S.op("pe", lambda: nc.tensor.transpose(C.bank(7)[0:8, 0:128], mq[:], C.identf[:]), reads=[G8, C.IDT], writes=[C.PS[7]])
                S.op("act", lambda: nc.scalar.copy(out=MTb[0:8, qt * 128:(qt + 1) * 128], in_=C.bank(7)[0:8, 0:128]),
                     reads=[C.PS[7]], writes=[MT])

            def extra(kt, qb, qs0, bank):
                n = kt // 2
                ops = []
                for qs in range(qs0, 4):
                    qt = 4 * qb + qs
                    if qt >= 8 and n < qt // 2:
                        ops.append((bank[:, qs * 128:(qs + 1) * 128], selb[:, n, :], MTb[:, qt * 128:(qt + 1) * 128]))
                return ops, [SEL, MT]

            def on_O(qt, oap, otok, h=h):
                rc = stt[:, qt:qt + 1]
                S.op("dve", lambda: nc.vector.reciprocal(out=rc, in_=oap[:, 128:129]), reads=[otok], writes=[STT])
                S.op("dve", lambda: nc.vector.tensor_scalar(out=mo[:, qt, h * 128:(h + 1) * 128], in0=oap[:, 0:128], scalar1=rc, scalar2=None, op0=ALU.mult),
                     reads=[otok, STT], writes=[MO])
            attention(C, ptl, "mb%d" % h, qT, kT, QK, vb, VB, 128, T0b, T1b, TBK, exp_transform(C, scale), on_O, extra=extra)
        junk = sb("junk", [128, 1024]); JK = S.buf("mb_junk")
        yo = [sb("yo%d" % i, [128, 1024]) for i in range(2)]; YO = S.bufs("mb_yo", 2)
        for qt in range(16):
            ss = stt[:, 16 + qt:17 + qt]; sd = stt[:, 32 + qt:33 + qt]; rs = stt[:, 48 + qt:49 + qt]
            S.op("act", lambda: nc.scalar.activation(out=junk[:], in_=mo[:, qt, :], func=AF.Square, accum_out=ss), reads=[MO], writes=[JK, STT])
            rstd_ops(C, ss, sd, rs, 1024, STT)
            y = yo[qt % 2]; Y = YO[qt % 2]
            S.op("dve", lambda: nc.vector.scalar_tensor_tensor(out=y[:], in0=mo[:, qt, :], scalar=rs, in1=monw[:], op0=ALU.mult, op1=ALU.mult),
                 reads=[MO, STT, MW], writes=[Y])
            S.dma("sp", mixed_d[qt * 128:(qt + 1) * 128, 2048:3072], y[:], reads=[Y])
        S.barrier()


def ssd_mixer(C, projF, projT, negmask_d, convw_d, convb_d, dtb_d, alog_d, dsk_d, nw_d, acs_d, mixed_d):
    nc, S = C.nc, C.S
    with ExitStack() as st:
        sb = lambda n, s, d=F32: st.enter_context(_sbt(nc, "sd_" + n, s, d))
        negm = sb("negm", [128, 128]); NM = S.buf("sd_negm")
        S.dma("sp", negm[:], negmask_d[:, :], writes=[NM])
        cw = sb("cw", [128, 16, 4]); cb = sb("cb", [128, 16]); CW = S.buf("sd_cw")
        with nc.allow_non_contiguous_dma(reason="small param transposes"):
            for j in range(4):
                S.dma("sp", cw[:, :, j], convw_d[j, 0, :].rearrange("(c p) -> p c", p=128), writes=[CW])
            S.dma("sp", cb[:], convb_d.rearrange("(c p) -> p c", p=128), writes=[CW])
        dsk = sb("dsk", [128, 16]); DSK = S.buf("sd_dsk")
        S.dma("sp", dsk[:], dsk_d.partition_broadcast(128), writes=[DSK])
        nwb = sb("nwb", [128, 1024]); NWB = S.buf("sd_nwb")
        S.dma("sp", nwb[:], nw_d.partition_broadcast(128), writes=[NWB])
        xtm = sb("xtm", [128, 16, 1024]); XTM = S.buf("sd_xtm")
        bc = sb("bc", [128, 8, 2048], BF16); BCK = S.buf("sd_bc")
        vbx = sb("vbx", [128, 16, 16, 64], BF16); VBX = S.buf("sd_vbx")
        dtT = sb("dtT", [128, 16, 16]); nacT = sb("nacT", [128, 16, 16]); DTT = S.buf("sd_dtT")
        with ExitStack() as st2:
            sb2 = lambda n, s, d=F32: st2.enter_context(_sbt(nc, "sd2_" + n, s, d))
            dtr = sb2("dtr", [16, 2048]); dtv = sb2("dtv", [16, 2048]); av = sb2("av", [16, 2048]); ones = sb2("ones", [16, 2048])
            acu = sb2("acu", [16, 2048]); sm = sb2("sm", [16, 4]); DTK = S.buf("sd_dtk"); SM = S.buf("sd_sm")
            with nc.allow_non_contiguous_dma(reason="small param columns"):
                S.dma("sp", sm[:, 0:1], dtb_d.rearrange("(h o) -> h o", o=1), writes=[SM])
                S.dma("sp", sm[:, 1:2], alog_d.rearrange("(h o) -> h o", o=1), writes=[SM])
            S.op("act", lambda: nc.scalar.activation(out=sm[:, 2:3], in_=sm[:, 1:2], func=AF.Exp), reads=[SM], writes=[SM])
            S.op("dve", lambda: nc.vector.tensor_scalar(out=sm[:, 3:4], in0=sm[:, 2:3], scalar1=-1.0, scalar2=None, op0=ALU.mult),
                 reads=[SM], writes=[SM])
            S.dma("sp", dtr[:], projF[10240:10256, :], writes=[DTK])
            S.op("act", lambda: nc.scalar.activation(out=dtv[:], in_=dtr[:], func=AF.Exp, bias=sm[:, 0:1], scale=1.0),
                 reads=[DTK, SM], writes=[DTK])
            S.op("act", lambda: nc.scalar.activation(out=dtv[:], in_=dtv[:], func=AF.Ln, bias=1.0, scale=1.0), reads=[DTK], writes=[DTK])
            S.op("dve", lambda: nc.vector.tensor_scalar(out=av[:], in0=dtv[:], scalar1=sm[:, 3:4], scalar2=None, op0=ALU.mult),
                 reads=[DTK, SM], writes=[DTK])
            S.op("pool", lambda: nc.gpsimd.memset(ones[:], 1.0), writes=[DTK])
            S.op("dve", lambda: nc.vector.tensor_tensor_scan(out=acu[:], data0=ones[:], data1=av[:], initial=0.0, op0=ALU.mult, op1=ALU.add),
                 reads=[DTK], writes=[DTK])
            S.dma("sp", acs_d[:, :], acu[:], reads=[DTK], writes=[DTK])
            for tt in range(16):
                S.op("pe", lambda: nc.tensor.transpose(C.bank(6)[:, 0:16], dtv[:, tt * 128:(tt + 1) * 128], C.identf[0:16, 0:16]),
                     reads=[DTK, C.IDT], writes=[C.PS[6]])
                S.op("dve", lambda: nc.vector.tensor_copy(out=dtT[:, tt, :], in_=C.bank(6)[:, 0:16]), reads=[C.PS[6]], writes=[DTT])
                S.op("pe", lambda: nc.tensor.transpose(C.bank(7)[:, 0:16], acu[:, tt * 128:(tt + 1) * 128], C.identf[0:16, 0:16]),
                     reads=[DTK, C.IDT], writes=[C.PS[7]])
                S.op("act", lambda: nc.scalar.mul(out=nacT[:, tt, :], in_=C.bank(7)[:, 0:16], mul=-1.0), reads=[C.PS[7]], writes=[DTT])
            S.barrier()
        with ExitStack() as st2:
            sb2 = lambda n, s, d=F32: st2.enter_context(_sbt(nc, "sd3_" + n, s, d))
            xin = [sb2("xin%d" % i, [128, 2048]) for i in range(2)]; XIN = S.bufs("sd_xin", 2)
            acc = [sb2("acc%d" % i, [128, 2048]) for i in range(2)]; ACC = S.bufs("sd_acc", 2)
            for c in range(16):
                i = c % 2
                X = xin[i]; A = acc[i]
                S.dma("sp", X[:], projF[8192 + c * 128:8192 + (c + 1) * 128, :], writes=[XIN[i]])
                S.op("dve", lambda: nc.vector.tensor_scalar(out=A[:], in0=X[:], scalar1=cw[:, c, 3:4], scalar2=cb[:, c:c + 1],
                                                            op0=ALU.mult, op1=ALU.add), reads=[XIN[i], CW], writes=[ACC[i]])
                for sft in (1, 2, 3):
                    S.op("dve", lambda: nc.vector.scalar_tensor_tensor(out=A[:, sft:2048], in0=X[:, 0:2048 - sft], scalar=cw[:, c, 3 - sft:4 - sft],
                                                                       in1=A[:, sft:2048], op0=ALU.mult, op1=ALU.add),
                         reads=[XIN[i], CW, ACC[i]], writes=[ACC[i]])
                if c < 8:
                    S.op("act", lambda: nc.scalar.activation(out=A[:], in_=A[:], func=AF.Silu), reads=[ACC[i]], writes=[ACC[i]])
                    for t4 in range(4):
                        b = 6 + (t4 % 2)
                        fns = []
                        for k in range(4):
                            tt = t4 * 4 + k
                            fns.append(lambda k=k, tt=tt: nc.tensor.transpose(C.bank(b)[:, k * 128:(k + 1) * 128], A[:, tt * 128:(tt + 1) * 128], C.identf[:]))
                        S.group("pe", fns, reads=[ACC[i], C.IDT], writes=[C.PS[b]])
                        S.op("dve", lambda: nc.vector.tensor_copy(out=xtm[:, t4 * 4:(t4 + 1) * 4, c * 128:(c + 1) * 128],
                                                                  in_=C.bank(b).rearrange("p (k q) -> p k q", k=4)), reads=[C.PS[b]], writes=[XTM])
                else:
                    S.op("act", lambda: nc.scalar.activation(out=bc[:, c - 8, :], in_=A[:], func=AF.Silu), reads=[ACC[i]], writes=[BCK])
            S.barrier()
        for tt in range(16):
            eng = "dve" if tt % 2 == 0 else "pool"
            f = (nc.vector if eng == "dve" else nc.gpsimd)
            S.op(eng, lambda: f.tensor_tensor(out=vbx[:, tt, :, :], in0=xtm[:, tt, :].rearrange("p (h d) -> p h d", d=64),
                                              in1=dtT[:, tt, :].unsqueeze(2).to_broadcast([128, 16, 64]), op=ALU.mult),
                 reads=[XTM, DTT], writes=[VBX])
        acb = [sb("acb%d" % i, [128, 2048]) for i in range(2)]; ACB = S.bufs("sd_acb", 2)
        et = [sb("et%d" % i, [128, 512]) for i in range(3)]; ET = S.bufs("sd_et", 3)
        dgl = [sb("dgl%d" % i, [128, 16, 128]) for i in range(2)]; DGL = S.bufs("sd_dgl", 2)
        ptl = ([sb("pt%d" % i, [128, 512], BF16) for i in range(4)], S.bufs("sd_pt", 4))
        cnt = [0]
        for h in range(16):
            g = h // 4
            ab = acb[h % 2]; AB = ACB[h % 2]
            S.dma("sp", ab[:], acs_d[h, :].partition_broadcast(128), writes=[AB])
            S.op("dve", lambda: nc.vector.tensor_tensor(out=dgl[h % 2][:], in0=ab[:].rearrange("p (t k) -> p t k", k=128),
                                                        in1=negm[:].unsqueeze(1).to_broadcast([128, 16, 128]), op=ALU.add),
                 reads=[AB, NM], writes=[DGL[h % 2]])

            def transform(kt, qb, c0, pap, oap, reads, writes, h=h, ab=ab, AB=AB):
                e = et[cnt[0] % 3]; E = ET[cnt[0] % 3]
                cnt[0] += 1
                nb = nacT[:, kt, h:h + 1]
                q0 = qb * 512
                lo = c0
                if kt >= 4 * qb:
                    S.op("act", lambda: nc.scalar.activation(out=e[:, c0:c0 + 128], in_=dgl[h % 2][:, kt, :], func=AF.Exp, bias=nb, scale=1.0),
                         reads=[DGL[h % 2], DTT], writes=[E])
                    lo = c0 + 128
                if lo < 512:
                    S.op("act", lambda: nc.scalar.activation(out=e[:, lo:512], in_=ab[:, q0 + lo:q0 + 512], func=AF.Exp, bias=nb, scale=1.0),
                         reads=[AB, DTT], writes=[E])
                S.op("dve", lambda: nc.vector.tensor_tensor(out=oap, in0=pap, in1=e[:, c0:512], op=ALU.mult), reads=reads + [E], writes=writes)

            def on_O(qt, oap, otok, h=h):
                S.op("dve", lambda: nc.vector.scalar_tensor_tensor(out=xtm[:, qt, h * 64:(h + 1) * 64], in0=xtm[:, qt, h * 64:(h + 1) * 64],
                                                                   scalar=dsk[:, h:h + 1], in1=oap, op0=ALU.mult, op1=ALU.add),
                     reads=[otok, XTM, DSK], writes=[XTM])
            attention(C, ptl, "sd%d" % h, bc[:, 4 + g, :], bc[:, g, :], BCK, vbx[:, :, h, :], VBX, 64, None, None, None, transform, on_O)
        zt = [sb("zt%d" % i, [128, 1024]) for i in range(2)]; ZT = S.bufs("sd_zt", 2)
        junk = sb("junk", [128, 256]); JK = S.buf("sd_junk")
        stt = sb("stt", [128, 16 * 12]); STT = S.buf("sd_stt")
        for qt in range(16):
            z = zt[qt % 2]; Z = ZT[qt % 2]
            S.dma("sp", z[:], projT[qt * 128:(qt + 1) * 128, 7168:8192], writes=[Z])
            S.op("act", lambda: nc.scalar.activation(out=z[:], in_=z[:], func=AF.Silu), reads=[Z], writes=[Z])
            S.op("dve", lambda: nc.vector.tensor_tensor(out=z[:], in0=z[:], in1=xtm[:, qt, :], op=ALU.mult), reads=[Z, XTM], writes=[Z])
            for g in range(4):
                c = qt * 12 + g * 3
                ss = stt[:, c:c + 1]; sd = stt[:, c + 1:c + 2]; rs = stt[:, c + 2:c + 3]
                S.op("act", lambda: nc.scalar.activation(out=junk[:], in_=z[:, g * 256:(g + 1) * 256], func=AF.Square, accum_out=ss),
                     reads=[Z], writes=[JK, STT])
                rstd_ops(C, ss, sd, rs, 256, STT)
                S.op("dve", lambda: nc.vector.scalar_tensor_tensor(out=z[:, g * 256:(g + 1) * 256], in0=z[:, g * 256:(g + 1) * 256], scalar=rs,
                                                                   in1=nwb[:, g * 256:(g + 1) * 256], op0=ALU.mult, op1=ALU.mult),
                     reads=[Z, STT, NWB], writes=[Z])
            S.dma("sp", mixed_d[qt * 128:(qt + 1) * 128, 3072:4096], z[:], reads=[Z])
        S.barrier()


TWO_PI = 6.283185307179586


def s5_mixer(C, projF, prm, iota_d, m2_d, m3_d, m4_d, gtm_d, mixed_d, gTd=None, tabd=None):
    nc, S = C.nc, C.S
    V, G = nc.vector, nc.gpsimd

    def tt(eng, out, in0, in1, op, reads, writes):
        f = V if eng == "dve" else G
        return S.op(eng, lambda: f.tensor_tensor(out=out, in0=in0, in1=in1, op=op), reads=reads, writes=writes)

    def ts(eng, out, in0, s1, op0, reads, writes, s2=None, op1=None):
        f = V if eng == "dve" else G
        if op1 is None:
            return S.op(eng, lambda: f.tensor_scalar(out=out, in0=in0, scalar1=s1, scalar2=None, op0=op0), reads=reads, writes=writes)
        return S.op(eng, lambda: f.tensor_scalar(out=out, in0=in0, scalar1=s1, scalar2=s2, op0=op0, op1=op1), reads=reads, writes=writes)

    def wrap_half(eng, r, m, RK):
        ts(eng, m, r, 0.5, ALU.is_gt, [RK], [RK])
        tt(eng, r, r, m, ALU.subtract, [RK], [RK])
        ts(eng, m, r, -0.5, ALU.is_lt, [RK], [RK])
        tt(eng, r, r, m, ALU.add, [RK], [RK])

    def frac_reduce(eng, kf, ki, r, m, RK):
        S.op("dve", lambda: V.tensor_copy(out=ki, in_=kf), reads=[RK], writes=[RK])
        S.op("dve", lambda: V.tensor_copy(out=r, in_=ki), reads=[RK], writes=[RK])
        tt(eng, r, kf, r, ALU.subtract, [RK], [RK])
        wrap_half(eng, r, m, RK)

    SINS = TWO_PI * (1.0 - 2e-6)
    with ExitStack() as st:
        stp = ExitStack()
        GT = S.buf("s5_gT")
        sb = lambda n, s, d=F32: stp.enter_context(_sbt(nc, "s5_" + n, s, d))
        mag = sb("mag", [128, 32]); thn = sb("thn", [128, 32]); dcol = sb("dcol", [128, 8]); iot = sb("iot", [128, 2048])
        LTzr = [sb("LTzr%d" % k, [128, 8, 128], BF16) for k in range(4)]; LTzi = [sb("LTzi%d" % k, [128, 8, 128], BF16) for k in range(4)]
        Lzr = [sb("Lzr%d" % k, [128, 8, 128], BF16) for k in range(4)]; Lzi = [sb("Lzi%d" % k, [128, 8, 128], BF16) for k in range(4)]
        sts = ExitStack()
        sb = lambda n, s, d=F32: sts.enter_context(_sbt(nc, "s5_" + n, s, d))
        P = S.buf("s5_prm")
        lr = sb("lr", [128, 32]); li = sb("li", [128, 32]); ldt = sb("ldt", [128, 32])
        with nc.allow_non_contiguous_dma(reason="small param layouts"):
            S.dma("sp", lr[:], prm["lam_re"].rearrange("g p -> (g p)").rearrange("(j q) -> q j", q=128), writes=[P])
            S.dma("sp", li[:], prm["lam_im"].rearrange("g p -> (g p)").rearrange("(j q) -> q j", q=128), writes=[P])
            for g2 in range(2):
                S.dma("sp", ldt[g2 * 64:(g2 + 1) * 64, :], prm["log_dt"].rearrange("(j g) -> g j", g=2)[g2, :].partition_broadcast(64), writes=[P])
        dtv = sb("dtv", [128, 32])
        ki32 = sb("ki32", [128, 32], I32); r0 = sb("r0", [128, 32]); m0 = sb("m0", [128, 32]); rc0 = sb("rc0", [128, 32])
        sn0 = sb("sn0", [128, 32]); cs0 = sb("cs0", [128, 32])
        S.op("act", lambda: nc.scalar.activation(out=dtv[:], in_=ldt[:], func=AF.Exp), reads=[P], writes=[P])
        tt("dve", mag[:], lr[:], dtv[:], ALU.mult, [P], [P])
        S.op("act", lambda: nc.scalar.activation(out=mag[:], in_=mag[:], func=AF.Exp), reads=[P], writes=[P])
        tt("dve", thn[:], li[:], dtv[:], ALU.mult, [P], [P])
        ts("dve", thn[:], thn[:], 1.0 / TWO_PI, ALU.mult, [P], [P])
        frac_reduce("dve", thn[:], ki32[:], r0[:], m0[:], P)
        S.op("act", lambda: nc.scalar.activation(out=sn0[:], in_=r0[:], func=AF.Sin, scale=SINS), reads=[P], writes=[P])
        ts("dve", rc0[:], r0[:], 0.25, ALU.add, [P], [P])
        ts("dve", m0[:], rc0[:], 0.5, ALU.is_gt, [P], [P])
        tt("dve", rc0[:], rc0[:], m0[:], ALU.subtract, [P], [P])
        S.op("act", lambda: nc.scalar.activation(out=cs0[:], in_=rc0[:], func=AF.Sin, scale=SINS), reads=[P], writes=[P])
        abr = sb("abr", [128, 32]); abi = sb("abi", [128, 32]); den = sb("den", [128, 32]); t0 = sb("t0", [128, 32]); t1 = sb("t1", [128, 32])
        fre = sb("fre", [128, 32]); fim = sb("fim", [128, 32])
        tt("dve", abr[:], mag[:], cs0[:], ALU.mult, [P], [P])
        ts("dve", abr[:], abr[:], -1.0, ALU.add, [P], [P])
        tt("dve", abi[:], mag[:], sn0[:], ALU.mult, [P], [P])
        tt("dve", den[:], lr[:], lr[:], ALU.mult, [P], [P])
        tt("dve", t0[:], li[:], li[:], ALU.mult, [P], [P])
        tt("dve", den[:], den[:], t0[:], ALU.add, [P], [P])
        S.op("dve", lambda: V.reciprocal(out=den[:], in_=den[:]), reads=[P], writes=[P])
        tt("dve", t0[:], abr[:], lr[:], ALU.mult, [P], [P])
        tt("dve", t1[:], abi[:], li[:], ALU.mult, [P], [P])
        tt("dve", fre[:], t0[:], t1[:], ALU.add, [P], [P])
        tt("dve", fre[:], fre[:], den[:], ALU.mult, [P], [P])
        tt("dve", t0[:], abi[:], lr[:], ALU.mult, [P], [P])
        tt("dve", t1[:], abr[:], li[:], ALU.mult, [P], [P])
        tt("dve", fim[:], t0[:], t1[:], ALU.subtract, [P], [P])
        tt("dve", fim[:], fim[:], den[:], ALU.mult, [P], [P])
        br = sb("br", [128, 32, 16]); bi = sb("bi", [128, 32, 16]); bbr = sb("bbr", [128, 32, 16]); bbi = sb("bbi", [128, 32, 16])
        tb = sb("tb", [128, 32, 16])
        S.dma("sp", br[:], prm["b_re"].rearrange("g p h -> (g p h)").rearrange("(j q h) -> q j h", q=128, h=16), writes=[P])
        S.dma("sp", bi[:], prm["b_im"].rearrange("g p h -> (g p h)").rearrange("(j q h) -> q j h", q=128, h=16), writes=[P])
        fre_b = fre[:].unsqueeze(2).to_broadcast([128, 32, 16]); fim_b = fim[:].unsqueeze(2).to_broadcast([128, 32, 16])
        tt("dve", bbr[:], br[:], fre_b, ALU.mult, [P], [P])
        tt("dve", tb[:], bi[:], fim_b, ALU.mult, [P], [P])
        tt("dve", bbr[:], bbr[:], tb[:], ALU.subtract, [P], [P])
        tt("dve", bbi[:], bi[:], fre_b, ALU.mult, [P], [P])
        tt("dve", tb[:], br[:], fim_b, ALU.mult, [P], [P])
        tt("dve", bbi[:], bbi[:], tb[:], ALU.add, [P], [P])
        m2 = sb("m2", [128, 2]); m3 = sb("m3", [128, 2])
        S.dma("sp", m2[:], m2_d[:, :], writes=[P])
        S.dma("sp", m3[:], m3_d[:, :], writes=[P])
        LTr = sb("LTr", [128, 8, 128], BF16); LTi = sb("LTi", [128, 8, 128], BF16); LTK = S.buf("s5_LT")
        bx = sb("bx", [128, 4, 2, 16], BF16); BX = S.buf("s5_bx")
        for (src, dst) in ((bbr, LTr), (bbi, LTi)):
            for c in range(8):
                S.op("dve", lambda: V.tensor_tensor(out=bx[:], in0=src[:, 4 * c:4 * c + 4, :].unsqueeze(2).to_broadcast([128, 4, 2, 16]),
                                                    in1=m2[:].unsqueeze(1).unsqueeze(3).to_broadcast([128, 4, 2, 16]), op=ALU.mult),
                     reads=[P], writes=[BX])
                pb = C.bank_bf(7)
                S.op("pe", lambda: nc.tensor.transpose(pb[:, 0:128], bx[:].rearrange("p a b c -> p (a b c)"), C.ident[:]),
                     reads=[BX, C.IDT], writes=[C.PS[7]])
                S.op("act", lambda: nc.scalar.copy(out=dst[:, c, :], in_=pb[:, 0:128]), reads=[C.PS[7]], writes=[LTK])
        crn = sb("crn", [128, 8, 64]); cin = sb("cin", [128, 8, 64])
        S.dma("sp", crn[:], prm["c_re"].rearrange("g h p -> (g h p)").rearrange("(c q p) -> q c p", q=128, p=64), writes=[P])
        S.dma("sp", cin[:], prm["c_im"].rearrange("g h p -> (g h p)").rearrange("(c q p) -> q c p", q=128, p=64), writes=[P])
        Lr = sb("Lr", [128, 8, 128], BF16); Li = sb("Li", [128, 8, 128], BF16); LCK = S.buf("s5_LC")
        cx = sb("cx", [128, 2, 64], BF16); CX = S.buf("s5_cx")
        for (src, dst, sgn) in ((crn, Lr, 1.0), (cin, Li, -1.0)):
            for c in range(8):
                S.op("dve", lambda: V.tensor_tensor(out=cx[:], in0=src[:, c, :].unsqueeze(1).to_broadcast([128, 2, 64]),
                                                    in1=m3[:].unsqueeze(2).to_broadcast([128, 2, 64]), op=ALU.mult), reads=[P], writes=[CX])
                pb = C.bank_bf(7)
                S.op("pe", lambda: nc.tensor.transpose(pb[:, 0:128], cx[:].rearrange("p a b -> p (a b)"), C.ident[:]),
                     reads=[CX, C.IDT], writes=[C.PS[7]])
                S.op("act", lambda: nc.scalar.mul(out=dst[:, c, :], in_=pb[:, 0:128], mul=sgn), reads=[C.PS[7]], writes=[LCK])
        m4 = sb("m4", [128, 4])
        S.dma("sp", m4[:], m4_d[:, :], writes=[P])
        for k in range(4):
            for (src, dst) in ((LTr, LTzr[k]), (LTi, LTzi[k])):
                S.op("dve", lambda: V.tensor_scalar(out=dst[:], in0=src[:], scalar1=m4[:, k:k + 1], scalar2=None, op0=ALU.mult), reads=[LTK, P], writes=[LTK])
            for (src, dst) in ((Lr, Lzr[k]), (Li, Lzi[k])):
                S.op("pool", lambda: G.memset(dst[:], 0.0), writes=[LCK])
                S.op("dve", lambda: V.tensor_copy(out=dst[:, :, 32 * k:32 * k + 32], in_=src[:, :, 32 * k:32 * k + 32]), reads=[LCK], writes=[LCK])
        with nc.allow_non_contiguous_dma(reason="small"):
            S.dma("sp", dcol[:], prm["d"].rearrange("g h -> (g h)").rearrange("(c q) -> q c", q=128), writes=[P])
        IOT = S.buf("s5_iot")
        S.dma("sp", iot[:], iota_d.partition_broadcast(128), writes=[IOT])
        S.barrier()
        sts.close()
        with ExitStack() as st2:
            sb2 = lambda n, s, d=F32: st2.enter_context(_sbt(nc, "s5b_" + n, s, d))
            ufl = [sb2("uf%d" % i, [128, 2048]) for i in range(2)]; ubl = [sb2("ub%d" % i, [128, 2048], BF16) for i in range(2)]; UFL = S.bufs("s5_uf", 2)
            cosT = sb2("cosT", [128, 2048]); sinT = sb2("sinT", [128, 2048]); TBL = S.buf("s5_tbl")
            x1 = sb2("x1", [128, 2048]); x2 = sb2("x2", [128, 2048]); x3 = sb2("x3", [128, 2048]); x4 = sb2("x4", [128, 2048])
            X1, X2, X3, X4 = S.bufs("s5_x", 4)
            bre = sb2("bre", [128, 2048]); bim = sb2("bim", [128, 2048]); BRE = S.buf("s5_bre"); BIM = S.buf("s5_bim")
            sre = sb2("sre", [128, 4, 2048], BF16); sim = sb2("sim", [128, 4, 2048], BF16); SK = S.buf("s5_s")
            yT = sb2("yT", [128, 2048]); YK = S.buf("s5_yT")
            gx = sb2("gx", [128, 2048]); XK = S.buf("s5_gx")
            gb = sb2("gb", [128, 2048], BF16); GB = S.buf("s5_gb")
            gst = Stager(C, st2, "s5_gst", 1)
            HALF_PI = 1.5707963267948966

            cosL = [cosT, sb2("cosT1", [128, 2048])]; sinL = [sinT, sb2("sinT1", [128, 2048])]
            TBLS = [TBL, S.buf("s5_tbl1")]

            def A_load(j):
                S.dma("sp", cosL[j % 2][:], tabd[j, 0, :, :], writes=[TBLS[j % 2]])
                S.dma("sp", sinL[j % 2][:], tabd[j, 1, :, :], writes=[TBLS[j % 2]])

            def A_dve(j):
                pass

            def A_act(j):
                if j + 1 < 32:
                    A_load(j + 1)

            A_load(0)
            A_load(1)
            for c in range(8):
                uf = ufl[c % 2]; ub = ubl[c % 2]; UF = UFL[c % 2]
                S.dma("sp", uf[:], projF[c * 128:(c + 1) * 128, :], writes=[UF])
                S.op("act", lambda: nc.scalar.copy(out=ub[:], in_=uf[:]), reads=[UF], writes=[UF])
                for jj in range(4):
                    j = 4 * c + jj
                    fns = []
                    for tb_ in range(4):
                        for (LT, b0) in ((LTzr[jj], 0), (LTzi[jj], 4)):
                            fns.append(lambda tb_=tb_, LT=LT, b0=b0: nc.tensor.matmul(
                                C.bank(b0 + tb_), LT[:, c, :], ub[:, tb_ * 512:(tb_ + 1) * 512],
                                start=True, stop=True))
                    S.group("pe", fns, reads=[LTK, UF], writes=C.PS)
                    pre = C.ps[:, 0:2048]; pim = C.ps[:, 2048:4096]
                    cosT = cosL[j % 2]; sinT = sinL[j % 2]; TBL = TBLS[j % 2]
                    tt("dve", x1[:], pre, cosT[:], ALU.mult, C.PS + [TBL], [X1])
                    tt("dve", x2[:], pim, sinT[:], ALU.mult, C.PS + [TBL], [X2])
                    tt("dve", x3[:], pim, cosT[:], ALU.mult, C.PS + [TBL], [X3])
                    tt("dve", x4[:], pre, sinT[:], ALU.mult, C.PS + [TBL], [X4])
                    tt("pool", bre[:], x1[:], x2[:], ALU.subtract, [X1, X2], [BRE])
                    tt("pool", bim[:], x3[:], x4[:], ALU.add, [X3, X4], [BIM])
                    if j + 1 < 32:
                        A_dve(j + 1)
                    magb = mag[:, j:j + 1].to_broadcast([128, 2048])
                    S.op("dve", lambda: V.tensor_tensor_scan(out=bre[:], data0=magb, data1=bre[:], initial=0.0, op0=ALU.mult, op1=ALU.add),
                         reads=[BRE, P], writes=[BRE])
                    S.op("dve", lambda: V.tensor_tensor_scan(out=bim[:], data0=magb, data1=bim[:], initial=0.0, op0=ALU.mult, op1=ALU.add),
                         reads=[BIM, P], writes=[BIM])
                    tt("dve", x1[:], bre[:], cosT[:], ALU.mult, [BRE, TBL], [X1])
                    tt("pool", x4[:], bre[:], sinT[:], ALU.mult, [BRE, TBL], [X4])
                    tt("dve", x3[:], bim[:], cosT[:], ALU.mult, [BIM, TBL], [X3])
                    tt("pool", x2[:], bim[:], sinT[:], ALU.mult, [BIM, TBL], [X2])
                    tt("dve", sre[:, jj, :], x1[:], x2[:], ALU.add, [X1, X2], [SK])
                    tt("pool", sim[:, jj, :], x3[:], x4[:], ALU.subtract, [X3, X4], [SK])
                    if j + 1 < 32:
                        A_act(j + 1)
                for tb_ in range(4):
                    b = tb_ % 2
                    fns = []
                    for jj in range(4):
                        fns.append(lambda jj=jj: nc.tensor.matmul(C.bank(b), Lzr[jj][:, c, :],
                                                                  sre[:, jj, tb_ * 512:(tb_ + 1) * 512], start=(jj == 0), stop=False))
                        fns.append(lambda jj=jj: nc.tensor.matmul(C.bank(b), Lzi[jj][:, c, :],
                                                                  sim[:, jj, tb_ * 512:(tb_ + 1) * 512], start=False, stop=(jj == 3)))
                    S.group("pe", fns, reads=[LCK, SK], writes=[C.PS[b]])
                    S.op("dve", lambda: V.scalar_tensor_tensor(out=yT[:, tb_ * 512:(tb_ + 1) * 512], in0=uf[:, tb_ * 512:(tb_ + 1) * 512],
                                                               scalar=dcol[:, c:c + 1], in1=C.bank(b), op0=ALU.mult, op1=ALU.add),
                         reads=[UF, P, C.PS[b]], writes=[YK])
                tt("pool", gx[:], yT[:], yT[:], ALU.mult, [YK], [XK])
                ts("pool", gx[:], gx[:], 0.044715, ALU.mult, [XK], [XK], s2=1.0, op1=ALU.add)
                tt("pool", gx[:], gx[:], yT[:], ALU.mult, [XK, YK], [XK])
                S.op("act", lambda: nc.scalar.activation(out=gx[:], in_=gx[:], func=AF.Sigmoid, scale=1.5957691216), reads=[XK], writes=[XK])
                tt("pool", yT[:], yT[:], gx[:], ALU.mult, [XK, YK], [YK])
                S.op("act", lambda: nc.scalar.copy(out=gb[:], in_=yT[:]), reads=[YK], writes=[GB])
                S.dma("pool", gTd[c * 128:(c + 1) * 128, :], gb[:], reads=[GB])
                for t4 in range(4):
                    b = 2 + (t4 % 2)
                    fns = []
                    for k in range(4):
                        tq = t4 * 4 + k
                        fns.append(lambda k=k, tq=tq: nc.tensor.transpose(C.bank(b)[:, k * 128:(k + 1) * 128], yT[:, tq * 128:(tq + 1) * 128], C.identf[:]))
                    S.group("pe", fns, reads=[YK, C.IDT], writes=[C.PS[b]])
                    g_, gk, eng = gst.next()
                    copy_op(C, eng, g_[:], C.bank(b), [C.PS[b]], [gk])
                    S.dma("sp", gtm_d[t4 * 512:(t4 + 1) * 512, c * 128:(c + 1) * 128].rearrange("(k p) n -> p k n", p=128),
                          g_[:].rearrange("p (k n) -> p k n", k=4), reads=[gk])
            S.barrier()
        stp.close()
        with ExitStack() as st2:
            sb2 = lambda n, s, d=F32: st2.enter_context(_sbt(nc, "s5c_" + n, s, d))
            otm = sb2("otm", [128, 16, 1024]); OT = S.buf("s5_otm")
            onw = sb2("onw", [128, 1024]); ONW = S.buf("s5_onw")
            S.dma("sp", onw[:], prm["onw"].partition_broadcast(128), writes=[ONW])
            sg = Stager(C, st2, "s5_sg", 2)
            gi = Stager(C, st2, "s5_gi", 2)
            for half in range(2):
                def evac(mode, c0, t_, pap, ptok, half=half):
                    tq = half * 8 + t_
                    s_, sk, _ = sg.next()
                    g_, gk, _ = gi.next()
                    S.dma("sp", g_[:], gtm_d[tq * 128:(tq + 1) * 128, c0:c0 + 512], writes=[gk])
                    S.op("act", lambda: nc.scalar.activation(out=s_[:], in_=pap, func=AF.Sigmoid), reads=[ptok], writes=[sk])
                    S.op("dve", lambda: V.tensor_tensor(out=otm[:, tq, c0:c0 + 512], in0=s_[:], in1=g_[:], op=ALU.mult), reads=[sk, gk], writes=[OT])
                gTh = sb2("gTh%d" % half, [128, 8, 1024], BF16)
                S.dma("sp", gTh[:], gTd.rearrange("(c p) t -> p c t", p=128)[:, :, half * 1024:(half + 1) * 1024], writes=[GT])
                dense(C, gTh, GT, 8, 1024, prm["w_glu"], [(0, 512, "tm"), (512, 512, "tm")], evac, name="glu%d" % half)
            junk = sb2("junk", [128, 1024]); JK = S.buf("s5_junk")
            stt = sb2("stt", [128, 64]); STT = S.buf("s5_stt")
            yo = [sb2("yo%d" % i, [128, 1024]) for i in range(2)]; YO = S.bufs("s5_yo", 2)
            for qt in range(16):
                ss = stt[:, qt:qt + 1]; sd = stt[:, 16 + qt:17 + qt]; rs = stt[:, 32 + qt:33 + qt]
                S.op("act", lambda: nc.scalar.activation(out=junk[:], in_=otm[:, qt, :], func=AF.Square, accum_out=ss), reads=[OT], writes=[JK, STT])
                rstd_ops(C, ss, sd, rs, 1024, STT)
                y = yo[qt % 2]; Y = YO[qt % 2]
                S.op("dve", lambda: V.scalar_tensor_tensor(out=y[:], in0=otm[:, qt, :], scalar=rs, in1=onw[:], op0=ALU.mult, op1=ALU.mult),
                     reads=[OT, STT, ONW], writes=[Y])
                S.dma("sp", mixed_d[qt * 128:(qt + 1) * 128, 0:1024], y[:], reads=[Y])
            S.barrier()


class S5TableGen:
    def __init__(self, C, st, lam_im_d, log_dt_d, iota_d, tabd):
        nc, S = C.nc, C.S
        self.C = C
        sb = lambda n, s, d=F32: st.enter_context(_sbt(nc, "tg_" + n, s, d))
        self.thn = sb("thn", [128, 32]); li = sb("li", [128, 32]); ldt = sb("ldt", [128, 32])
        self.P = S.buf("tg_p")
        with nc.allow_non_contiguous_dma(reason="small param layouts"):
            S.dma("sp", li[:], lam_im_d.rearrange("g p -> (g p)").rearrange("(j q) -> q j", q=128), writes=[self.P])
            for g2 in range(2):
                S.dma("sp", ldt[g2 * 64:(g2 + 1) * 64, :], log_dt_d.rearrange("(j g) -> g j", g=2)[g2, :].partition_broadcast(64), writes=[self.P])
        S.op("act", lambda: nc.scalar.activation(out=ldt[:], in_=ldt[:], func=AF.Exp), reads=[self.P], writes=[self.P])
        S.op("dve", lambda: nc.vector.tensor_tensor(out=self.thn[:], in0=li[:], in1=ldt[:], op=ALU.mult), reads=[self.P], writes=[self.P])
        S.op("dve", lambda: nc.vector.tensor_scalar(out=self.thn[:], in0=self.thn[:], scalar1=1.0 / TWO_PI, scalar2=None, op0=ALU.mult),
             reads=[self.P], writes=[self.P])
        self.iot = sb("iot", [128, 2048]); self.IOT = S.buf("tg_iot")
        S.dma("sp", self.iot[:], iota_d.partition_broadcast(128), writes=[self.IOT])
        self.kf = sb("kf", [128, 2048]); self.ki = sb("ki", [128, 2048], I32); self.rb = sb("rb", [128, 2048])
        self.RK = S.buf("tg_rk"); self.RB = S.buf("tg_rb")
        self.cosb = [sb("cos%d" % i, [128, 2048]) for i in range(2)]; self.sinb = [sb("sin%d" % i, [128, 2048]) for i in range(2)]
        self.CB = S.bufs("tg_cb", 2); self.SB = S.bufs("tg_sb", 2)
        self.tabd = tabd
        self.gen = self._run()
        self.done = False

    def _run(self):
        C = self.C
        nc, S = C.nc, C.S
        V = nc.vector
        SINS = TWO_PI * (1.0 - 2e-6)
        HALF_PI = 1.5707963267948966
        kf, ki, rb = self.kf, self.ki, self.rb
        RK, RB = self.RK, self.RB
        for j in range(32):
            b = j % 2
            S.op("dve", lambda: V.tensor_scalar(out=kf[:], in0=self.iot[:], scalar1=self.thn[:, j:j + 1], scalar2=8.0, op0=ALU.mult, op1=ALU.add),
                 reads=[self.IOT, self.P], writes=[RK])
            yield
            S.op("dve", lambda: V.tensor_copy(out=ki[:], in_=kf[:]), reads=[RK], writes=[RK])
            yield
            S.op("dve", lambda: V.tensor_tensor(out=rb[:], in0=kf[:], in1=ki[:], op=ALU.subtract), reads=[RK], writes=[RB])
            yield
            S.op("dve", lambda: V.scalar_tensor_tensor(out=kf[:], in0=rb[:], scalar=0.5, in1=rb[:], op0=ALU.is_ge, op1=ALU.subtract),
                 reads=[RB, RK], writes=[RK])
            yield
            S.op("act", lambda: nc.scalar.activation(out=self.sinb[b][:], in_=kf[:], func=AF.Sin, scale=SINS), reads=[RK], writes=[self.SB[b]])
            yield
            S.op("act", lambda: nc.scalar.activation(out=rb[:], in_=kf[:], func=AF.Abs), reads=[RK], writes=[RB])
            yield
            S.op("act", lambda: nc.scalar.activation(out=self.cosb[b][:], in_=rb[:], func=AF.Sin, scale=-SINS, bias=HALF_PI * (1.0 - 2e-6)),
                 reads=[RB], writes=[self.CB[b]])
            yield
            S.dma("pool", self.tabd[j, 0, :, :], self.cosb[b][:], reads=[self.CB[b]])
            S.dma("pool", self.tabd[j, 1, :, :], self.sinb[b][:], reads=[self.SB[b]])
            yield

    def step(self):
        if self.done:
            return
        try:
            next(self.gen)
        except StopIteration:
            self.done = True

    def drain(self):
        while not self.done:
            self.step()


SEQ = 2048
PARAM_SHAPES = {
    "rel_bias_table": [32, 16], "attn_norm_w": [2, 4096], "w_in": [2, 4096, 10256], "s5_lam_re": [2, 64, 64],
    "s5_lam_im": [2, 64, 64], "s5_log_dt": [2, 64], "s5_b_re": [2, 64, 64, 16], "s5_b_im": [2, 64, 64, 16],
    "s5_c_re": [2, 64, 16, 64], "s5_c_im": [2, 64, 16, 64], "s5_d": [2, 64, 16], "s5_w_glu": [2, 1024, 1024],
    "s5_out_norm_w": [2, 1024], "diff_lam_q1": [2, 64], "diff_lam_k1": [2, 64], "diff_lam_q2": [2, 64],
    "diff_lam_k2": [2, 64], "diff_subln_w": [2, 128], "moba_out_norm_w": [2, 1024], "ssd_conv_w": [2, 4, 1, 2048],
    "ssd_conv_b": [2, 2048], "ssd_dt_bias": [2, 16], "ssd_a_log": [2, 16], "ssd_d": [2, 16], "ssd_norm_w": [2, 1024],
    "w_out": [2, 4096, 4096], "mlp_norm_w": [2, 4096], "w_up": [2, 4096, 16384], "w_down": [2, 16384, 4096],
    "final_norm_w": [4096]}
CONST_SHAPES = {"iota": [2048], "m2c": [128, 2], "m3c": [128, 2], "m4c": [128, 4], "ohm": [2, 32, 16384],
                "negmask": [128, 128], "ident_d": [128, 128], "sel": [8, 1024]}


def build_program(depth=2):
    nc = bass.Bass("TRN2", target_bir_lowering=False)
    dt = lambda n, s, k="ExternalInput", d=F32: nc.dram_tensor(n, s, d, kind=k).ap()
    x = dt("x", [SEQ, D])
    p = {k: dt(k, s) for k, s in PARAM_SHAPES.items()}
    c = {k: dt(k, s) for k, s in CONST_SHAPES.items()}
    out = dt("out", [SEQ, D], "ExternalOutput")
    projF = dt("projF", [INW, SEQ], "Internal"); projT = dt("projT", [SEQ, INW], "Internal")
    mixed = dt("mixed", [SEQ, D], "Internal"); xmid = dt("xmid", [SEQ, D], "Internal")
    xs_ = [dt("xres%d" % i, [SEQ, D], "Internal") for i in range(2)]
    act = dt("act", [16384, 1024], "Internal", BF16)
    btd = dt("btd", [2, 16, 16384], "Internal"); acs = dt("acs", [16, 2048], "Internal"); gtm = dt("gtm", [2048, 1024], "Internal"); gTd = dt("gTd", [1024, 2048], "Internal", BF16); tabd = dt("tabd", [32, 2, 128, 2048], "Internal")
    with ExitStack() as st:
        C = Ctx(nc, st)
        load_consts(C, c["ident_d"])
        build_bias(C, p["rel_bias_table"], c["ohm"], btd)
        for l in range(depth):
            x_in = x if l == 0 else xs_[(l - 1) % 2]
            x_out = xs_[l % 2]
            with ExitStack() as stg_:
                tg = S5TableGen(C, stg_, p["s5_lam_im"][l], p["s5_log_dt"][l], c["iota"], tabd)
                for half in range(2):
                    phase_A(C, x_in[half * 1024:(half + 1) * 1024, :], p["attn_norm_w"][l], p["w_in"][l], 1024, half * 1024, projF, projT,
                            bg=tg.step)
                tg.drain()
                C.S.barrier()
            s5p = {"lam_re": p["s5_lam_re"][l], "lam_im": p["s5_lam_im"][l], "log_dt": p["s5_log_dt"][l], "b_re": p["s5_b_re"][l],
                   "b_im": p["s5_b_im"][l], "c_re": p["s5_c_re"][l], "c_im": p["s5_c_im"][l], "d": p["s5_d"][l],
                   "w_glu": p["s5_w_glu"][l], "onw": p["s5_out_norm_w"][l]}
            s5_mixer(C, projF, s5p, c["iota"], c["m2c"], c["m3c"], c["m4c"], gtm, mixed, gTd, tabd=tabd)
            diff_attn(C, l, projF, projT, btd, c["negmask"],
                      [p["diff_lam_q1"][l], p["diff_lam_k1"][l], p["diff_lam_q2"][l], p["diff_lam_k2"][l]], p["diff_subln_w"][l], mixed)
            moba_attn(C, projF, projT, btd, c["negmask"], c["sel"], p["moba_out_norm_w"][l], mixed)
            ssd_mixer(C, projF, projT, c["negmask"], p["ssd_conv_w"][l], p["ssd_conv_b"][l], p["ssd_dt_bias"][l], p["ssd_a_log"][l],
                      p["ssd_d"][l], p["ssd_norm_w"][l], acs, mixed)
            last = (l == depth - 1)
            for half in range(2):
                phase_C(C, None, x_in, p["w_out"][l], p["mlp_norm_w"][l], p["w_up"][l], p["w_down"][l], 1024, half * 1024,
                        xmid, act, x_out, p["final_norm_w"] if last else None, out if last else None, mixed_tm=mixed)
        C.S.barrier()
    return nc


def kernel(**inputs):
    nc = build_program()
    consts = host_consts()
    params = {k: np.ascontiguousarray(np.asarray(inputs[k], dtype=np.float32)) for k in PARAM_SHAPES}
    x = np.asarray(inputs["x"], dtype=np.float32)
    B = x.shape[0]
    in_maps = []
    for b in range(B):
        m = {"x": np.ascontiguousarray(x[b])}
        m.update(params)
        m.update(consts)
        in_maps.append(m)
    res = run_bass_kernel_spmd(nc, in_maps, core_ids=list(range(B)))
    return np.stack([np.asarray(res.results[b]["out"]) for b in range(B)], axis=0).astype(np.float32)
```
